# Optimizing a Trainium2 kernel written in Bass

```python
import math
import jax, jax.numpy as jnp
from jax import lax
import numpy as np

D_MODEL = 1024
BATCH = 4
SEQ = 8192
DEPTH = 4

N_BRANCH = 4
BRANCH_W = 256
GM_GROUPS = 4
GM_CHUNK = 128
GM_GD = BRANCH_W // GM_GROUPS
POOL_WINDOWS = (2, 4, 8, 16)
POOL_GD = BRANCH_W // len(POOL_WINDOWS)
ATT_HEADS = 4
ATT_HD = BRANCH_W // ATT_HEADS
IDX_HEADS = 4
IDX_HD = 32
TOPK_MAX = 256
Q_BLOCK = 128
REL_BUCKETS = 32
REL_MAX_DIST = 128
CONV_K = 31
D_FF = 2816
EPS = 1e-6

IN_SIZES = (2 * BRANCH_W, BRANCH_W, BRANCH_W, BRANCH_W, BRANCH_W, IDX_HEADS * IDX_HD, IDX_HD, IDX_HEADS, 2 * BRANCH_W, N_BRANCH * D_MODEL)
IN_COLS = 2 * BRANCH_W + 4 * BRANCH_W + IDX_HEADS * IDX_HD + IDX_HD + IDX_HEADS + 2 * BRANCH_W + N_BRANCH * D_MODEL

kernel_name = 'hybrid_gated_parallel_mixers'


def rmsnorm(x, g):
    xf = x.astype(jnp.float32)
    y = xf * lax.rsqrt(jnp.mean(xf * xf, axis=-1, keepdims=True) + EPS)
    return (y * g.astype(jnp.float32)).astype(x.dtype)


def layernorm(x, g, b):
    xf = x.astype(jnp.float32)
    mu = jnp.mean(xf, axis=-1, keepdims=True)
    xc = xf - mu
    y = xc * lax.rsqrt(jnp.mean(xc * xc, axis=-1, keepdims=True) + EPS)
    return (y * g.astype(jnp.float32) + b.astype(jnp.float32)).astype(x.dtype)


def swiglu_ffn(h, w_gu, w_down):
    a, b = jnp.split(h @ w_gu, 2, axis=-1)
    return (jax.nn.silu(a) * b) @ w_down


def gmlp_mixer(uv, v_g, ws, wb):
    B, S, _ = uv.shape
    u, v = jnp.split(jax.nn.gelu(uv), 2, axis=-1)
    v = rmsnorm(v, v_g)
    vc = v.reshape(B, S // GM_CHUNK, GM_CHUNK, GM_GROUPS, GM_GD)
    causal = jnp.tril(jnp.ones((GM_CHUNK, GM_CHUNK), dtype=bool))
    wsm = jnp.where(causal[None], ws, 0.0).astype(v.dtype)
    mixed = jnp.einsum('gts,bcsgd->bctgd', wsm, vc) + wb.T[None, None, :, :, None]
    return u * mixed.reshape(B, S, BRANCH_W)


def pool_mixer(p, pw, scale):
    B, S, _ = p.shape
    wmax = POOL_WINDOWS[-1]
    pf = p.astype(jnp.float32)
    csp = jnp.pad(jnp.cumsum(pf, axis=1), ((0, 0), (wmax, 0), (0, 0)))
    count = jnp.arange(1, S + 1, dtype=jnp.float32)[:, None]
    outs = []
    for g, w in enumerate(POOL_WINDOWS):
        sl = slice(g * POOL_GD, (g + 1) * POOL_GD)
        win = csp[:, wmax:, sl] - csp[:, wmax - w:wmax - w + S, sl]
        outs.append(win / jnp.minimum(count, float(w)) - pf[:, :, sl])
    d = jnp.stack(outs, axis=2).astype(p.dtype)
    y = jnp.einsum('bsgc,gcd->bsgd', d, pw).reshape(B, S, BRANCH_W)
    return y * scale


def conv_mixer(ab, dw, db, ln_g, ln_b):
    a, gt = jnp.split(ab, 2, axis=-1)
    h = a * jax.nn.sigmoid(gt)
    h = lax.conv_general_dilated(h, dw[:, None, :].astype(h.dtype), window_strides=(1,),
                                 padding=[(CONV_K - 1, 0)], dimension_numbers=('NWC', 'WIO', 'NWC'),
                                 feature_group_count=BRANCH_W) + db
    return jax.nn.silu(layernorm(h, ln_g, ln_b))


def t5_bucket(n):
    n = jnp.maximum(n, 0)
    max_exact = REL_BUCKETS // 2
    large = max_exact + (jnp.log(jnp.maximum(n, 1).astype(jnp.float32) / max_exact)
                         / math.log(REL_MAX_DIST / max_exact) * (REL_BUCKETS - max_exact)).astype(jnp.int32)
    return jnp.where(n < max_exact, n, jnp.minimum(large, REL_BUCKETS - 1))


def sparse_attention(q, k, v, q_idx, k_idx, w_idx, rel_bias):
    B, S, _ = q.shape
    topk = min(TOPK_MAX, S // 4)
    q = q.reshape(B, S, ATT_HEADS, ATT_HD)
    k = k.reshape(B, S, ATT_HEADS, ATT_HD)
    v = v.reshape(B, S, ATT_HEADS, ATT_HD)
    q_idx = q_idx.reshape(B, S, IDX_HEADS, IDX_HD)
    w_idx = w_idx.astype(jnp.float32) * (IDX_HEADS ** -0.5)
    s_pos = jnp.arange(S)
    gather = jax.vmap(lambda a, i: a[i])

    def block(i):
        t0 = i * Q_BLOCK
        t_pos = t0 + jnp.arange(Q_BLOCK)
        qb = lax.dynamic_slice_in_dim(q, t0, Q_BLOCK, axis=1)
        qib = lax.dynamic_slice_in_dim(q_idx, t0, Q_BLOCK, axis=1)
        wib = lax.dynamic_slice_in_dim(w_idx, t0, Q_BLOCK, axis=1)
        rel = jax.nn.relu(jnp.einsum('bqhd,bsd->bqhs', qib, k_idx).astype(jnp.float32) * (IDX_HD ** -0.5))
        score = jnp.einsum('bqh,bqhs->bqs', wib, rel)
        score = jnp.where(s_pos[None, None, :] <= t_pos[None, :, None], score, -jnp.inf)
        _, idx = lax.top_k(score, topk)
        valid = idx <= t_pos[None, :, None]
        ks = gather(k, idx)
        vs = gather(v, idx)
        bias = rel_bias[t5_bucket(t_pos[None, :, None] - idx)]
        logits = (jnp.einsum('bqhd,bqkhd->bqhk', qb, ks).astype(jnp.float32) * (ATT_HD ** -0.5)
                  + bias.astype(jnp.float32).transpose(0, 1, 3, 2))
        logits = jnp.where(valid[:, :, None, :], logits, -jnp.inf)
        p = jax.nn.softmax(logits, axis=-1).astype(v.dtype)
        o = jnp.einsum('bqhk,bqkhd->bqhd', p, vs)
        return o.reshape(B, Q_BLOCK, BRANCH_W)

    out = lax.map(block, jnp.arange(S // Q_BLOCK))
    return out.transpose(1, 0, 2, 3).reshape(B, S, BRANCH_W)


def mixing_sublayer(h, w_in, gm_v_g, gm_ws, gm_b, pool_w, pool_scale, conv_dw, conv_b,
                    conv_ln_g, conv_ln_b, w_branch, w_out, rel_bias):
    B, S, _ = h.shape
    z = h @ w_in
    uv, pz, qz, kz, vz, qi, ki, wi, cz, gz = jnp.split(z, list(np.cumsum(IN_SIZES)[:-1]), axis=-1)
    branches = (
        gmlp_mixer(uv, gm_v_g, gm_ws, gm_b),
        pool_mixer(pz, pool_w, pool_scale),
        sparse_attention(qz, kz, vz, qi, ki, wi, rel_bias),
        conv_mixer(cz, conv_dw, conv_b, conv_ln_g, conv_ln_b),
    )
    gates = jax.nn.sigmoid(gz.reshape(B, S, N_BRANCH, D_MODEL))
    y = gates[:, :, 0] * (branches[0] @ w_branch[0])
    for n in range(1, N_BRANCH):
        y = y + gates[:, :, n] * (branches[n] @ w_branch[n])
    return y @ w_out


def setup_inputs(seed: int = 0) -> dict:
    key = jax.random.key(seed)
    ks = jax.random.split(key, 24)
    f32 = jnp.float32

    def nrm(k, shape, scale):
        return jax.random.normal(k, shape, f32) * scale

    def gain(k, shape):
        return 1.0 + 0.05 * jax.random.normal(k, shape, f32)

    L, D, C, F = DEPTH, D_MODEL, BRANCH_W, D_FF
    return {
        'x': nrm(ks[0], (BATCH, SEQ, D), 1.0),
        'ffn1_pre_g': gain(ks[1], (L, D)),
        'ffn1_post_g': gain(ks[2], (L, D)),
        'ffn1_w_gu': nrm(ks[3], (L, D, 2 * F), D ** -0.5),
        'ffn1_w_down': nrm(ks[4], (L, F, D), F ** -0.5),
        'mix_pre_g': gain(ks[5], (L, D)),
        'mix_post_g': gain(ks[6], (L, D)),
        'w_in': nrm(ks[7], (L, D, IN_COLS), D ** -0.5),
        'gm_v_g': gain(ks[8], (L, C)),
        'gm_ws': nrm(ks[9], (L, GM_GROUPS, GM_CHUNK, GM_CHUNK), GM_CHUNK ** -0.5),
        'gm_b': 1.0 + 0.1 * jax.random.normal(ks[10], (L, GM_GROUPS, GM_CHUNK), f32),
        'pool_w': nrm(ks[11], (L, len(POOL_WINDOWS), POOL_GD, POOL_GD), POOL_GD ** -0.5),
        'pool_scale': gain(ks[12], (L, C)),
        'conv_dw': nrm(ks[13], (L, CONV_K, C), CONV_K ** -0.5),
        'conv_b': nrm(ks[14], (L, C), 0.02),
        'conv_ln_g': gain(ks[15], (L, C)),
        'conv_ln_b': nrm(ks[16], (L, C), 0.02),
        'w_branch': nrm(ks[17], (L, N_BRANCH, C, D), C ** -0.5),
        'w_out': nrm(ks[18], (L, D, D), D ** -0.5),
        'ffn2_pre_g': gain(ks[19], (L, D)),
        'ffn2_post_g': gain(ks[20], (L, D)),
        'ffn2_w_gu': nrm(ks[21], (L, D, 2 * F), D ** -0.5),
        'ffn2_w_down': nrm(ks[22], (L, F, D), F ** -0.5),
        'rel_bias': nrm(ks[23], (REL_BUCKETS, ATT_HEADS), 0.5),
    }


def reference(x, ffn1_pre_g, ffn1_post_g, ffn1_w_gu, ffn1_w_down, mix_pre_g, mix_post_g, w_in,
              gm_v_g, gm_ws, gm_b, pool_w, pool_scale, conv_dw, conv_b, conv_ln_g, conv_ln_b,
              w_branch, w_out, ffn2_pre_g, ffn2_post_g, ffn2_w_gu, ffn2_w_down, rel_bias):
    for l in range(DEPTH):
        h = swiglu_ffn(rmsnorm(x, ffn1_pre_g[l]), ffn1_w_gu[l], ffn1_w_down[l])
        x = x + 0.5 * rmsnorm(h, ffn1_post_g[l])
        h = mixing_sublayer(rmsnorm(x, mix_pre_g[l]), w_in[l], gm_v_g[l], gm_ws[l], gm_b[l],
                            pool_w[l], pool_scale[l], conv_dw[l], conv_b[l], conv_ln_g[l],
                            conv_ln_b[l], w_branch[l], w_out[l], rel_bias)
        x = x + rmsnorm(h, mix_post_g[l])
        h = swiglu_ffn(rmsnorm(x, ffn2_pre_g[l]), ffn2_w_gu[l], ffn2_w_down[l])
        x = x + 0.5 * rmsnorm(h, ffn2_post_g[l])
    return x
```

```python
import numpy as np
import ml_dtypes
from contextlib import ExitStack
import concourse.bass as bass
import concourse.mybir as mybir
from concourse.bass_utils import run_bass_kernel_spmd

F32 = mybir.dt.float32
BF16 = mybir.dt.bfloat16
AF = mybir.ActivationFunctionType
ALU = mybir.AluOpType
AX = mybir.AxisListType
NPBF = ml_dtypes.bfloat16

D = 1024
SEQ = 8192
NB = 4
DEPTH = 4
DFF = 2816
NFC = DFF // 128
IN_COLS = 6308
TC = 4096
NGRP = TC // 512
EPS = 1e-6
NCORES = 8


class Buf:
    def __init__(self, name, t=None):
        self.name = name
        self.t = t
        self.w = None
        self.r = []

    def __getitem__(self, k):
        return self.t[k]


class Sched:
    ENG = ("pe", "act", "dve", "pool", "sp")

    def __init__(self, nc, es):
        self.nc = nc
        self.es = es
        self.e = {"pe": nc.tensor, "act": nc.scalar, "dve": nc.vector, "pool": nc.gpsimd, "sp": nc.sync}
        self.sems = {}
        self.cnt = {}
        self.seen = {e: {} for e in self.ENG}
        for e in self.ENG:
            self.sems[e] = es.enter_context(nc.semaphore("c_" + e))
            self.cnt[e] = 0
        self.nchan = 0
        self.n_ins = 0

    def chan(self):
        k = "ch%d" % self.nchan
        self.nchan += 1
        self.sems[k] = self.es.enter_context(self.nc.semaphore(k))
        self.cnt[k] = 0
        return k

    def sb(self, name, shape, dt, es=None):
        t = (es or self.es).enter_context(self.nc.sbuf_tensor(name, list(shape), dt))
        return Buf(name, t)

    def ps(self, name, shape, dt, es=None):
        t = (es or self.es).enter_context(self.nc.psum_tensor(name, list(shape), dt))
        return Buf(name, t)

    def _waits(self, eng, reads, writes):
        evs = []
        for b in reads:
            if b.w is not None:
                evs.append(b.w)
        for b in writes:
            if b.w is not None:
                evs.append(b.w)
            evs.extend(b.r)
        waits = {}
        for (k, v) in evs:
            if k == "pe" and eng == "pe":
                continue
            if self.seen[eng].get(k, 0) >= v:
                continue
            waits[k] = max(waits.get(k, 0), v)
        for k, v in waits.items():
            self.seen[eng][k] = v
            self.e[eng].wait_ge(self.sems[k], v)

    def _mark(self, ev, reads, writes):
        for b in reads:
            b.r.append(ev)
            if len(b.r) > 12:
                d = {}
                for (k, v) in b.r:
                    d[k] = max(d.get(k, 0), v)
                b.r = list(d.items())
        for b in writes:
            b.w = ev
            b.r = []

    def op(self, eng, fn, reads=(), writes=()):
        self._waits(eng, reads, writes)
        self.cnt[eng] += 1
        ev = (eng, self.cnt[eng])
        fn(self.e[eng]).then_inc(self.sems[eng], 1)
        self.n_ins += 1
        self._mark(ev, reads, writes)
        return ev

    def dma(self, q, ch, out, in_, reads=(), writes=(), **kw):
        self._waits(q, reads, writes)
        self.cnt[ch] += 16
        ev = (ch, self.cnt[ch])
        self.e[q].dma_start(out=out, in_=in_, **kw).then_inc(self.sems[ch], 16)
        self.n_ins += 1
        self._mark(ev, reads, writes)
        return ev

    def seal(self, ch, bufs):
        for b in bufs:
            b.w = (ch, self.cnt[ch])

    def wait_bufs(self, eng, bufs):
        self._waits(eng, [], bufs)

    def barrier(self, bufs=()):
        for eng in self.ENG:
            for k in list(self.cnt.keys()):
                v = self.cnt[k]
                if v == 0 or (k == eng and eng == "pe"):
                    continue
                if self.seen[eng].get(k, 0) >= v:
                    continue
                self.seen[eng][k] = v
                self.e[eng].wait_ge(self.sems[k], v)


class Consts:
    pass


def make_consts(S, nc, es):
    C = Consts()
    C.onesD = S.sb("onesD", [128, 128], F32, es)
    S.op("pool", lambda e: e.memset(C.onesD[:], 1.0 / D), writes=[C.onesD])
    C.ones256 = S.sb("ones256", [128, 128], F32, es)
    S.op("pool", lambda e: e.memset(C.ones256[:], 1.0 / 256.0), writes=[C.ones256])
    C.eps = S.sb("epsc", [128, 1], F32, es)
    S.op("pool", lambda e: e.memset(C.eps[:], EPS), writes=[C.eps])
    C.ident = S.sb("ident", [128, 128], BF16, es)
    S.op("pool", lambda e: e.memset(C.ident[:], 1.0), writes=[C.ident])
    S.op("pool", lambda e: e.affine_select(out=C.ident[:], in_=C.ident[:], pattern=[[-1, 128]],
                                           compare_op=ALU.is_equal, fill=0.0, base=0, channel_multiplier=1),
         reads=[C.ident], writes=[C.ident])
    return C


def rstd_from_ms(S, ms_ps, rstd, C, n=512):
    S.op("act", lambda e: e.activation(out=rstd[:, :n], in_=ms_ps[:, :n], func=AF.Sqrt, bias=C.eps[:, 0:1], scale=1.0),
         reads=[ms_ps, C.eps], writes=[rstd])
    S.op("dve", lambda e: e.reciprocal(out=rstd[:, :n], in_=rstd[:, :n]), reads=[rstd], writes=[rstd])


def phase_ffn(S, nc, C, xT_d, xoT_d, preg_d, postg_d, wgu_d, wdn_d, ngrp=NGRP):
    xv = xT_d.rearrange("(kc p) t -> p kc t", p=128)
    xov = xoT_d.rearrange("(kc p) t -> p kc t", p=128)
    with ExitStack() as es:
        NX = 2
        xs = [S.sb("f_x%d" % i, [128, 8, 512], F32, es) for i in range(NX)]
        xch = [S.chan() for _ in range(NX)]
        xn = S.sb("f_xn", [128, 8, 512], BF16, es)
        sq = [S.sb("f_sq%d" % i, [128, 512], F32, es) for i in range(2)]
        act = S.sb("f_act", [128, NFC, 512], BF16, es)
        sl = [S.sb("f_sl%d" % i, [128, 512], F32, es) for i in range(2)]
        NW = 4
        wgu = [S.sb("f_wgu%d" % i, [128, 8, 256], BF16, es) for i in range(NW)]
        wch = [S.chan() for _ in range(NW)]
        wdn = S.sb("f_wdn", [128, NFC, 1024], BF16, es)
        wdch = S.chan()
        hT = S.sb("f_h", [128, 8, 512], F32, es)
        rstd = S.sb("f_rstd", [128, 512], F32, es)
        rstd2 = S.sb("f_rstd2", [128, 512], F32, es)
        tmp = [S.sb("f_tmp%d" % i, [128, 512], F32, es) for i in range(2)]
        pg = S.sb("f_pg", [128, 8], F32, es)
        qg = S.sb("f_qg", [128, 8], F32, es)
        gch = S.chan()
        och = S.chan()
        pA = [S.ps("f_pA%d" % i, [128, 512], F32, es) for i in range(2)]
        pB = [S.ps("f_pB%d" % i, [128, 512], F32, es) for i in range(2)]
        pO = [S.ps("f_pO%d" % i, [128, 512], F32, es) for i in range(2)]
        pS = S.ps("f_pS", [128, 512], F32, es)
        pS2 = S.ps("f_pS2", [128, 512], F32, es)

        S.dma("sp", gch, pg[:], preg_d, writes=[pg])
        S.dma("sp", gch, qg[:], postg_d, writes=[qg])
        S.seal(gch, [pg, qg])

        def load_x(g):
            b = xs[g % NX]
            S.dma("sp", xch[g % NX], b[:], xv[:, :, g * 512:(g + 1) * 512], writes=[b])

        load_x(0)
        wcount = [0]

        def load_w(fc):
            i = wcount[0] % NW
            wcount[0] += 1
            S.dma("sp", wch[i], wgu[i][:], wgu_d[fc], writes=[wgu[i]])
            return wgu[i]

        for g in range(ngrp):
            x = xs[g % NX]
            if g + 1 < ngrp:
                load_x(g + 1)
            pend = [load_w(0), load_w(1)]
            S.dma("act", wdch, wdn[:], wdn_d, writes=[wdn])
            for kc in range(8):
                s = sq[kc % 2]
                S.op("act", lambda e, s=s, kc=kc: e.activation(out=s[:], in_=x[:, kc, :], func=AF.Square),
                     reads=[x], writes=[s])
                S.op("pe", lambda e, s=s, kc=kc: e.matmul(pS[:], lhsT=C.onesD[:], rhs=s[:], start=(kc == 0), stop=(kc == 7)),
                     reads=[s, C.onesD], writes=[pS])
            rstd_from_ms(S, pS, rstd, C)
            for kc in range(8):
                S.op("dve", lambda e, kc=kc: e.scalar_tensor_tensor(out=xn[:, kc, :], in0=x[:, kc, :], scalar=pg[:, kc:kc + 1],
                                                                    in1=rstd[:], op0=ALU.mult, op1=ALU.mult),
                     reads=[x, pg, rstd], writes=[xn])
            for fc in range(NFC):
                w = pend.pop(0)
                if fc + 2 < NFC:
                    pend.append(load_w(fc + 2))
                A = pA[fc % 2]
                B = pB[fc % 2]
                for kc in range(8):
                    S.op("pe", lambda e, w=w, kc=kc, A=A: e.matmul(A[:], lhsT=w[:, kc, 0:128], rhs=xn[:, kc, :],
                                                                   start=(kc == 0), stop=(kc == 7)),
                         reads=[w, xn], writes=[A])
                for kc in range(8):
                    S.op("pe", lambda e, w=w, kc=kc, B=B: e.matmul(B[:], lhsT=w[:, kc, 128:256], rhs=xn[:, kc, :],
                                                                   start=(kc == 0), stop=(kc == 7)),
                         reads=[w, xn], writes=[B])
                s_ = sl[fc % 2]
                S.op("act", lambda e, s_=s_, A=A: e.activation(out=s_[:], in_=A[:], func=AF.Silu), reads=[A], writes=[s_])
                S.op("dve", lambda e, s_=s_, B=B, fc=fc: e.tensor_tensor(out=act[:, fc, :], in0=s_[:], in1=B[:], op=ALU.mult),
                     reads=[s_, B], writes=[act])
            for oc in range(8):
                O = pO[oc % 2]
                for fc in range(NFC):
                    S.op("pe", lambda e, O=O, fc=fc, oc=oc: e.matmul(O[:], lhsT=wdn[:, fc, oc * 128:(oc + 1) * 128],
                                                                     rhs=act[:, fc, :], start=(fc == 0), stop=(fc == NFC - 1)),
                         reads=[wdn, act], writes=[O])
                S.op("act", lambda e, O=O, oc=oc: e.copy(out=hT[:, oc, :], in_=O[:]), reads=[O], writes=[hT])
                s = sq[oc % 2]
                S.op("act", lambda e, s=s, O=O: e.activation(out=s[:], in_=O[:], func=AF.Square),
                     reads=[O], writes=[s])
                S.op("pe", lambda e, s=s, oc=oc: e.matmul(pS2[:], lhsT=C.onesD[:], rhs=s[:], start=(oc == 0), stop=(oc == 7)),
                     reads=[s, C.onesD], writes=[pS2])
            rstd_from_ms(S, pS2, rstd2, C)
            for oc in range(8):
                t = tmp[oc % 2]
                S.op("dve", lambda e, t=t, oc=oc: e.scalar_tensor_tensor(out=t[:], in0=hT[:, oc, :], scalar=qg[:, oc:oc + 1],
                                                                        in1=rstd2[:], op0=ALU.mult, op1=ALU.mult),
                     reads=[hT, qg, rstd2], writes=[t])
                S.op("dve", lambda e, t=t, oc=oc: e.scalar_tensor_tensor(out=x[:, oc, :], in0=t[:], scalar=0.5,
                                                                         in1=x[:, oc, :], op0=ALU.mult, op1=ALU.add),
                     reads=[t, x], writes=[x])
            S.dma("sp", xch[g % NX], xov[:, :, g * 512:(g + 1) * 512], x[:], reads=[x], writes=[])
        S.barrier()


def build_ffn_prog(ngrp=NGRP):
    nc = bass.Bass("TRN2", target_bir_lowering=False)
    T = ngrp * 512
    xT = nc.dram_tensor("xT", [D, T], F32, kind="ExternalInput").ap()
    preg = nc.dram_tensor("preg", [128, 8], F32, kind="ExternalInput").ap()
    postg = nc.dram_tensor("postg", [128, 8], F32, kind="ExternalInput").ap()
    wgu = nc.dram_tensor("wgu", [NFC, 128, 8, 256], BF16, kind="ExternalInput").ap()
    wdn = nc.dram_tensor("wdn", [128, NFC, 1024], BF16, kind="ExternalInput").ap()
    xoT = nc.dram_tensor("xoT", [D, T], F32, kind="ExternalOutput").ap()
    with ExitStack() as es:
        S = Sched(nc, es)
        C = make_consts(S, nc, es)
        phase_ffn(S, nc, C, xT, xoT, preg, postg, wgu, wdn, ngrp)
    return nc


def lay_gain(g):
    return np.ascontiguousarray(g.reshape(8, 128).T)


def lay_wgu(w):
    g = w[:, :DFF].reshape(8, 128, NFC, 128)
    u = w[:, DFF:].reshape(8, 128, NFC, 128)
    t = np.concatenate([g, u], axis=3)
    return np.ascontiguousarray(t.transpose(2, 1, 0, 3))


def lay_wdn(w):
    return np.ascontiguousarray(w.reshape(NFC, 128, D).transpose(1, 0, 2))


def phase_m1(S, nc, C, xT_d, preg_d, w1_d, wki_d, wv_d, hnT_d, kT_d, vtok_d, kiT_d, pT_d, hgT_d, ngrp=NGRP):
    xv = xT_d.rearrange("(kc p) t -> p kc t", p=128)
    hnv = hnT_d.rearrange("(kc p) t -> p kc t", p=128)
    kv = kT_d.rearrange("(c p) t -> p c t", p=128)
    pv = pT_d.rearrange("(c p) t -> p c t", p=128)
    hgv = hgT_d.rearrange("(c p) t -> p c t", p=128)
    with ExitStack() as es:
        xs = [S.sb("m1_x%d" % i, [128, 8, 512], F32, es) for i in range(2)]
        xch = [S.chan() for _ in range(2)]
        hn = [S.sb("m1_hn%d" % i, [128, 8, 512], BF16, es) for i in range(2)]
        hch = [S.chan() for _ in range(2)]
        sq = [S.sb("m1_sq%d" % i, [128, 512], F32, es) for i in range(2)]
        rstd = S.sb("m1_rstd", [128, 512], F32, es)
        w1 = S.sb("m1_w1", [128, 8, 1024], BF16, es)
        wki = S.sb("m1_wki", [128, 8, 32], BF16, es)
        wv = S.sb("m1_wv", [128, 8, 256], BF16, es)
        pg = S.sb("m1_pg", [128, 8], F32, es)
        wc = S.chan()
        kt = [S.sb("m1_kt%d" % i, [128, 2, 512], BF16, es) for i in range(2)]
        ktch = [S.chan() for _ in range(2)]
        pt = [S.sb("m1_pt%d" % i, [128, 2, 512], F32, es) for i in range(2)]
        ptch = [S.chan() for _ in range(2)]
        hg = [S.sb("m1_hg%d" % i, [128, 2, 512], F32, es) for i in range(2)]
        hgch = [S.chan() for _ in range(2)]
        sg = [S.sb("m1_sg%d" % i, [128, 512], F32, es) for i in range(2)]
        kit = [S.sb("m1_ki%d" % i, [32, 512], BF16, es) for i in range(2)]
        kich = [S.chan() for _ in range(2)]
        vt = [S.sb("m1_vt%d" % i, [128, 4, 256], BF16, es) for i in range(2)]
        vch = [S.chan() for _ in range(2)]
        pS = S.ps("m1_pS", [128, 512], F32, es)
        pP = [S.ps("m1_pP%d" % i, [128, 512], F32, es) for i in range(6)]

        S.dma("sp", wc, pg[:], preg_d, writes=[pg])
        S.dma("sp", wc, w1[:], w1_d, writes=[w1])
        S.dma("sp", wc, wki[:], wki_d, writes=[wki])
        S.dma("sp", wc, wv[:], wv_d, writes=[wv])
        S.seal(wc, [pg, w1, wki, wv])
        S.dma("sp", xch[0], xs[0][:], xv[:, :, 0:512], writes=[xs[0]])
        pi = [0]

        def nextp():
            p = pP[pi[0] % 6]
            pi[0] += 1
            return p

        for g in range(ngrp):
            x = xs[g % 2]
            h = hn[g % 2]
            tsl = slice(g * 512, (g + 1) * 512)
            if g + 1 < ngrp:
                S.dma("sp", xch[(g + 1) % 2], xs[(g + 1) % 2][:], xv[:, :, (g + 1) * 512:(g + 2) * 512], writes=[xs[(g + 1) % 2]])
            for kc in range(8):
                s = sq[kc % 2]
                S.op("act", lambda e, s=s, kc=kc: e.activation(out=s[:], in_=x[:, kc, :], func=AF.Square), reads=[x], writes=[s])
                S.op("pe", lambda e, s=s, kc=kc: e.matmul(pS[:], lhsT=C.onesD[:], rhs=s[:], start=(kc == 0), stop=(kc == 7)),
                     reads=[s, C.onesD], writes=[pS])
            rstd_from_ms(S, pS, rstd, C)
            for kc in range(8):
                S.op("dve", lambda e, kc=kc: e.scalar_tensor_tensor(out=h[:, kc, :], in0=x[:, kc, :], scalar=pg[:, kc:kc + 1],
                                                                    in1=rstd[:], op0=ALU.mult, op1=ALU.mult),
                     reads=[x, pg, rstd], writes=[h])
            S.dma("sp", hch[g % 2], hnv[:, :, tsl], h[:], reads=[h])

            def proj(oc, P):
                for kc in range(8):
                    S.op("pe", lambda e, kc=kc: e.matmul(P[:], lhsT=w1[:, kc, oc * 128:(oc + 1) * 128], rhs=h[:, kc, :],
                                                         start=(kc == 0), stop=(kc == 7)), reads=[w1, h], writes=[P])
            ktb = kt[g % 2]
            for c in range(2):
                P = nextp()
                proj(c, P)
                S.op("act", lambda e, c=c, P=P: e.copy(out=ktb[:, c, :], in_=P[:]), reads=[P], writes=[ktb])
            S.dma("sp", ktch[g % 2], kv[:, :, tsl], ktb[:], reads=[ktb])
            ptb = pt[g % 2]
            for c in range(2):
                P = nextp()
                proj(2 + c, P)
                S.op("dve", lambda e, c=c, P=P: e.tensor_copy(out=ptb[:, c, :], in_=P[:]), reads=[P], writes=[ptb])
            S.dma("sp", ptch[g % 2], pv[:, :, tsl], ptb[:], reads=[ptb])
            hgb = hg[g % 2]
            for c in range(2):
                Pa = nextp()
                proj(4 + c, Pa)
                Pg = nextp()
                proj(6 + c, Pg)
                s_ = sg[c]
                S.op("act", lambda e, s_=s_, Pg=Pg: e.activation(out=s_[:], in_=Pg[:], func=AF.Sigmoid), reads=[Pg], writes=[s_])
                S.op("dve", lambda e, c=c, s_=s_, Pa=Pa: e.tensor_tensor(out=hgb[:, c, :], in0=s_[:], in1=Pa[:], op=ALU.mult),
                     reads=[s_, Pa], writes=[hgb])
            S.dma("sp", hgch[g % 2], hgv[:, :, tsl], hgb[:], reads=[hgb])
            P = nextp()
            for kc in range(8):
                S.op("pe", lambda e, kc=kc: e.matmul(P[0:32, :], lhsT=wki[:, kc, :], rhs=h[:, kc, :], start=(kc == 0), stop=(kc == 7)),
                     reads=[wki, h], writes=[P])
            kib = kit[g % 2]
            S.op("act", lambda e, P=P: e.copy(out=kib[:], in_=P[0:32, :]), reads=[P], writes=[kib])
            S.dma("sp", kich[g % 2], kiT_d[:, tsl], kib[:], reads=[kib])
            vb = vt[g % 2]
            for tb in range(4):
                P = nextp()
                for kc in range(8):
                    S.op("pe", lambda e, kc=kc, tb=tb, P=P: e.matmul(P[:, 0:256], lhsT=h[:, kc, tb * 128:(tb + 1) * 128], rhs=wv[:, kc, :],
                                                                     start=(kc == 0), stop=(kc == 7)), reads=[wv, h], writes=[P])
                S.op("act", lambda e, tb=tb, P=P: e.copy(out=vb[:, tb, :], in_=P[:, 0:256]), reads=[P], writes=[vb])
            S.dma("sp", vch[g % 2], vtok_d[tsl, :].rearrange("(tb p) c -> p tb c", p=128), vb[:], reads=[vb])
        S.barrier()


def build_m1_prog(ngrp=NGRP):
    nc = bass.Bass("TRN2", target_bir_lowering=False)
    T = ngrp * 512
    xT = nc.dram_tensor("xT", [D, T], F32, kind="ExternalInput").ap()
    preg = nc.dram_tensor("preg", [128, 8], F32, kind="ExternalInput").ap()
    w1 = nc.dram_tensor("w1", [128, 8, 1024], BF16, kind="ExternalInput").ap()
    wki = nc.dram_tensor("wki", [128, 8, 32], BF16, kind="ExternalInput").ap()
    wv = nc.dram_tensor("wv", [128, 8, 256], BF16, kind="ExternalInput").ap()
    hnT = nc.dram_tensor("hnT", [D, T], BF16, kind="ExternalOutput").ap()
    kT = nc.dram_tensor("kT", [256, T], BF16, kind="ExternalOutput").ap()
    vtok = nc.dram_tensor("vtok", [T, 256], BF16, kind="ExternalOutput").ap()
    kiT = nc.dram_tensor("kiT", [32, T], BF16, kind="ExternalOutput").ap()
    pT = nc.dram_tensor("pT", [256, T], F32, kind="ExternalOutput").ap()
    hgT = nc.dram_tensor("hgT", [256, T], F32, kind="ExternalOutput").ap()
    with ExitStack() as es:
        S = Sched(nc, es)
        C = make_consts(S, nc, es)
        phase_m1(S, nc, C, xT, preg, w1, wki, wv, hnT, kT, vtok, kiT, pT, hgT, ngrp)
    return nc


def lay_kc(w):
    return np.ascontiguousarray(w.reshape(8, 128, w.shape[1]).transpose(1, 0, 2))


O_U, O_V, O_P, O_Q, O_K, O_VV, O_QI, O_KI, O_WI, O_A, O_GT, O_GZ = 0, 256, 512, 768, 1024, 1280, 1536, 1664, 1696, 1700, 1956, 2212


from concourse.bass_types import AP as RawAP

RBIS = 64.0
NIT = 31
DELTA = 2.0 ** -22
NJ = 32
NEG = -1.0e30


def phase_attn(S, nc, C, hnT_d, wq_d, wqi_d, wwi_d, kT_d, vf_d, kiT_d, relb_d, oh_d, vd_d, cm_d, coef_d, base_d,
               oT_d, ebd_t, nj=NJ):
    hnv = hnT_d.rearrange("(kc p) t -> p kc t", p=128)
    ov = oT_d.rearrange("(c p) t -> p c t", p=128)
    with ExitStack() as es:
        kT = S.sb("a_kT", [128, 2, SEQ], BF16, es)
        vx = S.sb("a_vx", [128, 64, 260], BF16, es)
        ki2 = S.sb("a_ki2", [64, SEQ], BF16, es)
        sacc = S.sb("a_sacc", [128, SEQ], F32, es)
        wq = S.sb("a_wq", [128, 8, 256], BF16, es)
        wqi = S.sb("a_wqi", [128, 8, 128], BF16, es)
        wwi = S.sb("a_wwi", [128, 8, 4], BF16, es)
        cm = S.sb("a_cm", [128, 2, 256], F32, es)
        coef = S.sb("a_coef", [128, 18], F32, es)
        base = S.sb("a_base", [128, 512], F32, es)
        rb = S.sb("a_rb", [32, 4], F32, es)
        oh = S.sb("a_oh", [32, 640], F32, es)
        vd = S.sb("a_vd", [128, 640], F32, es)
        rbh = S.sb("a_rbh", [32, 128], F32, es)
        eb = S.sb("a_eb", [128, 640], F32, es)
        negc = S.sb("a_negc", [128, 1], F32, es)
        nd = [S.sb("a_nd%d" % i, [128, 4, 128], F32, es) for i in range(2)]
        near = [[S.sb("a_near%d_%d" % (a, b), [128, 4, 128], F32, es) for b in range(3)] for a in range(2)]
        hnb = [S.sb("a_hn%d" % i, [128, 8, 128], BF16, es) for i in range(2)]
        hch = [S.chan() for _ in range(2)]
        qm = S.sb("a_qm", [128, 4, 128], BF16, es)
        qiA = S.sb("a_qiA", [64, 128], BF16, es)
        qiB = S.sb("a_qiB", [64, 128], BF16, es)
        wt = S.sb("a_wt", [128, 4], F32, es)
        tmp = [S.sb("a_tmp%d" % i, [128, 512], F32, es) for i in range(2)]
        lo = S.sb("a_lo", [128, 1], F32, es)
        mid = S.sb("a_mid", [128, 1], F32, es)
        cnt = S.sb("a_cnt", [128, 1], F32, es)
        stp = S.sb("a_stp", [128, 1], F32, es)
        junk = S.sb("a_junk", [128, SEQ], BF16, es)
        mk = [S.sb("a_mk%d" % i, [128, 128], BF16, es) for i in range(2)]
        E = [S.sb("a_E%d" % i, [128, 4, 128], BF16, es) for i in range(2)]
        Pm = [S.sb("a_P%d" % i, [128, 4, 128], BF16, es) for i in range(2)]
        rcp = S.sb("a_rcp", [128, 4], F32, es)
        o_n = S.sb("a_on", [128, 256], BF16, es)
        oTb = [S.sb("a_oT%d" % i, [128, 2, 128], BF16, es) for i in range(2)]
        och = [S.chan() for _ in range(2)]
        ldc = S.chan()
        ebc = S.chan()
        ndc = S.chan()
        SP = [S.ps("a_SP%d" % i, [128, 512], F32, es) for i in range(4)]
        PO = S.ps("a_PO", [128, 512], F32, es)
        PT = S.ps("a_PT", [128, 2, 128], BF16, es)
        PJ = [S.ps("a_PJ%d" % i, [128, 512], F32, es) for i in range(2)]
        OB = [PO, SP[2], SP[3], PJ[1]]

        S.dma("sp", ldc, kT[:], kT_d.rearrange("(c p) s -> p c s", p=128), writes=[kT])
        S.dma("sp", ldc, ki2[0:32, :], kiT_d, writes=[ki2])
        S.dma("sp", ldc, ki2[32:64, :], kiT_d, writes=[ki2])
        vfv = vf_d.rearrange("(k p) c -> p k c", p=128)
        for kk in range(8):
            S.dma("sp", ldc, vx[:, kk * 8:(kk + 1) * 8, :], vfv[:, kk * 8:(kk + 1) * 8, :], writes=[vx])
        for (t_, d_) in ((wq, wq_d), (wqi, wqi_d), (wwi, wwi_d), (cm, cm_d.rearrange("a p s -> p a s")), (coef, coef_d), (base, base_d),
                         (rb, relb_d), (oh, oh_d), (vd, vd_d)):
            S.dma("sp", ldc, t_[:], d_, writes=[t_])
        S.seal(ldc, [kT, ki2, vx, wq, wqi, wwi, cm, coef, base, rb, oh, vd])

        ebv = RawAP(ebd_t, 0, [[128 * 640, 4], [640, 128], [1, 640]])
        for h in range(4):
            S.op("dve", lambda e, h=h: e.tensor_copy(out=rbh[:], in_=rb[:, h:h + 1].to_broadcast([32, 128])), reads=[rb], writes=[rbh])
            S.op("pe", lambda e: e.matmul(PJ[0][:], lhsT=rbh[:], rhs=oh[:, 0:512], start=True, stop=True), reads=[rbh, oh], writes=[PJ[0]])
            S.op("pe", lambda e: e.matmul(PJ[1][:, 0:128], lhsT=rbh[:], rhs=oh[:, 512:640], start=True, stop=True), reads=[rbh, oh], writes=[PJ[1]])
            S.op("dve", lambda e: e.tensor_scalar(out=negc[:], in0=PJ[1][:, 127:128], scalar1=-1.0, scalar2=None, op0=ALU.mult),
                 reads=[PJ[1]], writes=[negc])
            S.op("act", lambda e: e.activation(out=eb[:, 0:512], in_=PJ[0][:], func=AF.Exp, bias=negc[:, 0:1], scale=1.0),
                 reads=[PJ[0], negc], writes=[eb])
            S.op("act", lambda e: e.activation(out=eb[:, 512:640], in_=PJ[1][:, 0:128], func=AF.Exp, bias=negc[:, 0:1], scale=1.0),
                 reads=[PJ[1], negc], writes=[eb])
            S.op("dve", lambda e: e.tensor_tensor(out=eb[:], in0=eb[:], in1=vd[:], op=ALU.mult), reads=[eb, vd], writes=[eb])
            S.dma("sp", ebc, ebv[h], eb[:], reads=[eb])
            ebuf = Buf("ebd")
            ebuf.w = (ebc, S.cnt[ebc])
            for dl in range(2):
                src = RawAP(ebd_t, h * 128 * 640 + 128 * dl + 127, [[639, 128], [1, 128]])
                S.dma("sp", ndc, nd[dl][:, h, :], src, reads=[ebuf], writes=[nd[dl]])
        S.seal(ndc, nd)
        for a in range(2):
            for b in range(3):
                t_ = near[a][b]
                ci = (a * 3 + b) * 3
                S.op("dve", lambda e, t_=t_, ci=ci: e.tensor_scalar(out=t_[:], in0=nd[0][:], scalar1=coef[:, ci:ci + 1], scalar2=None, op0=ALU.mult),
                     reads=[nd[0], coef], writes=[t_])
                S.op("dve", lambda e, t_=t_, ci=ci: e.scalar_tensor_tensor(out=t_[:], in0=nd[1][:], scalar=coef[:, ci + 1:ci + 2], in1=t_[:],
                                                                          op0=ALU.mult, op1=ALU.add), reads=[nd[1], coef, t_], writes=[t_])
                S.op("dve", lambda e, t_=t_, ci=ci: e.tensor_scalar(out=t_[:], in0=t_[:], scalar1=coef[:, ci + 2:ci + 3], scalar2=None, op0=ALU.add),
                     reads=[t_, coef], writes=[t_])

        S.op("pool", lambda e: e.memset(qm[:], 0.0), writes=[qm])
        S.dma("sp", hch[0], hnb[0][:], hnv[:, :, 0:128], writes=[hnb[0]])
        import os
        DBG = os.environ.get("KDBG", "")
        for j in range(nj if DBG != "tables" else 0):
            nk = 2 * j + 2
            L = nk * 128
            hb = hnb[j % 2]
            if j + 1 < nj:
                S.dma("sp", hch[(j + 1) % 2], hnb[(j + 1) % 2][:], hnv[:, :, (j + 1) * 128:(j + 2) * 128], writes=[hnb[(j + 1) % 2]])
            for c in range(2):
                P = PJ[c]
                for kc in range(8):
                    S.op("pe", lambda e, kc=kc, c=c, P=P: e.matmul(P[:, 0:128], lhsT=wq[:, kc, c * 128:(c + 1) * 128], rhs=hb[:, kc, :],
                                                                   start=(kc == 0), stop=(kc == 7)), reads=[wq, hb], writes=[P])
                S.op("act", lambda e, c=c, P=P: e.copy(out=qm[0:64, 2 * c, :], in_=P[0:64, 0:128]), reads=[P], writes=[qm])
                S.op("act", lambda e, c=c, P=P: e.copy(out=qm[64:128, 2 * c + 1, :], in_=P[64:128, 0:128]), reads=[P], writes=[qm])
            for c, qi_ in ((0, qiA), (1, qiB)):
                P = PJ[c]
                for kc in range(8):
                    S.op("pe", lambda e, kc=kc, c=c, P=P: e.matmul(P[0:64, 128:256], lhsT=wqi[:, kc, c * 64:(c + 1) * 64], rhs=hb[:, kc, :],
                                                                   start=(kc == 0), stop=(kc == 7)), reads=[wqi, hb], writes=[P])
                S.op("act", lambda e, qi_=qi_, P=P: e.copy(out=qi_[:], in_=P[0:64, 128:256]), reads=[P], writes=[qi_])
            P = PJ[0]
            for kc in range(8):
                S.op("pe", lambda e, kc=kc, P=P: e.matmul(P[:, 256:260], lhsT=hb[:, kc, :], rhs=wwi[:, kc, :], start=(kc == 0), stop=(kc == 7)),
                     reads=[wwi, hb], writes=[P])
            S.op("dve", lambda e, P=P: e.tensor_scalar(out=wt[:], in0=P[:, 256:260], scalar1=0.5 * (32.0 ** -0.5), scalar2=None, op0=ALU.mult),
                 reads=[P], writes=[wt])
            nch = (L + 511) // 512
            for c in range(nch):
                n = min(512, L - c * 512)
                ks = slice(c * 512, c * 512 + n)
                for h in range(4):
                    qi_ = qiA if h < 2 else qiB
                    pb = 32 * (h % 2)
                    S.op("pe", lambda e, h=h, qi_=qi_, pb=pb, ks=ks, n=n: e.matmul(SP[h][:, 0:n], lhsT=qi_[pb:pb + 32, :], rhs=ki2[pb:pb + 32, ks],
                                                                                 start=True, stop=True), reads=[qi_, ki2], writes=[SP[h]])
                    t_ = tmp[h % 2]
                    S.op("act", lambda e, h=h, t_=t_, n=n: e.activation(out=t_[:, 0:n], in_=SP[h][:, 0:n], func=AF.Relu), reads=[SP[h]], writes=[t_])
                    in1 = base[:, 0:n] if h == 0 else sacc[:, ks]
                    S.op("dve", lambda e, h=h, t_=t_, n=n, ks=ks, in1=in1: e.scalar_tensor_tensor(out=sacc[:, ks], in0=t_[:, 0:n], scalar=wt[:, h:h + 1],
                                                                                                 in1=in1, op0=ALU.mult, op1=ALU.add),
                         reads=[t_, wt, base, sacc], writes=[sacc])
                if c > 0:
                    S.op("pool", lambda e, ks=ks, c=c: e.tensor_scalar(out=sacc[:, ks], in0=sacc[:, ks], scalar1=-DELTA * 512.0 * c, scalar2=None, op0=ALU.add),
                         reads=[sacc], writes=[sacc])
            S.op("pool", lambda e, j=j, L=L: e.tensor_tensor(out=sacc[:, L - 256:L], in0=sacc[:, L - 256:L], in1=cm[:, j % 2, :], op=ALU.add),
                 reads=[sacc, cm], writes=[sacc])
            if DBG == "A":
                continue
            S.op("dve", lambda e: e.memset(lo[:], -RBIS), writes=[lo])
            for k in range(1, NIT + 1):
                wk = 2.0 * RBIS * (2.0 ** -k)
                S.op("dve", lambda e, wk=wk: e.tensor_scalar(out=mid[:], in0=lo[:], scalar1=wk, scalar2=None, op0=ALU.add), reads=[lo], writes=[mid])
                S.op("dve", lambda e, L=L: e.tensor_scalar(out=junk[:, 0:L], in0=sacc[:, 0:L], scalar1=mid[:, 0:1], scalar2=0.0, op0=ALU.is_ge, op1=ALU.add,
                                                          accum_out=cnt[:, 0:1]), reads=[sacc, mid], writes=[junk, cnt])
                S.op("dve", lambda e, wk=wk: e.tensor_scalar(out=stp[:], in0=cnt[:], scalar1=255.5, scalar2=wk, op0=ALU.is_ge, op1=ALU.mult),
                     reads=[cnt], writes=[stp])
                S.op("dve", lambda e: e.tensor_tensor(out=lo[:], in0=lo[:], in1=stp[:], op=ALU.add), reads=[lo, stp], writes=[lo])
            if DBG == "B":
                continue
            for kb in range(nk):
                m_ = mk[kb % 2]
                kb_s = slice(kb * 128, (kb + 1) * 128)
                S.op("dve", lambda e, m_=m_, kb_s=kb_s: e.tensor_scalar(out=m_[:], in0=sacc[:, kb_s], scalar1=lo[:, 0:1], scalar2=None, op0=ALU.is_ge),
                     reads=[sacc, lo], writes=[m_])
                S.op("pe", lambda e, m_=m_, kb=kb: e.transpose(PT[:, kb % 2, :], m_[:], C.ident[:]), reads=[m_, C.ident], writes=[PT])
                if DBG == "D1":
                    continue
                Lg = SP[kb % 2]
                for h in range(4):
                    pb = 64 * (h % 2)
                    S.op("pe", lambda e, h=h, pb=pb, kb_s=kb_s, Lg=Lg: e.matmul(Lg[:, h * 128:(h + 1) * 128], lhsT=kT[:, h // 2, kb_s],
                                                                               rhs=qm[:, h, :], start=True, stop=True),
                         reads=[kT, qm], writes=[Lg])
                E_ = E[kb % 2]
                S.op("act", lambda e, E_=E_, Lg=Lg: e.activation(out=E_[:].rearrange("p h t -> p (h t)"), in_=Lg[:], func=AF.Exp, scale=0.125),
                     reads=[Lg], writes=[E_])
                if DBG == "D2":
                    continue
                P_ = Pm[kb % 2]
                S.op("dve", lambda e, E_=E_, P_=P_, kb=kb: e.tensor_tensor(out=P_[:], in0=E_[:], in1=PT[:, kb % 2, :].unsqueeze(1).to_broadcast([128, 4, 128]),
                                                                          op=ALU.mult), reads=[E_, PT], writes=[P_])
                slot = kb - (2 * j - 1)
                if slot >= 0:
                    nr = near[j % 2][slot]
                    S.op("dve", lambda e, P_=P_, nr=nr: e.tensor_tensor(out=P_[:], in0=P_[:], in1=nr[:], op=ALU.mult), reads=[P_, nr], writes=[P_])
                if DBG == "D3":
                    continue
                for h in range(4):
                    S.op("pe", lambda e, h=h, P_=P_, kb=kb, nk=nk: e.matmul(OB[h][:, 0:65], lhsT=P_[:, h, :], rhs=vx[:, kb, h * 65:(h + 1) * 65],
                                                                           start=(kb == 0), stop=(kb == nk - 1)), reads=[P_, vx], writes=[OB[h]])
            if DBG in ("D", "D1", "D2", "D3"):
                continue
            for h in range(4):
                S.op("dve", lambda e, h=h: e.reciprocal(out=rcp[:, h:h + 1], in_=OB[h][:, 64:65]), reads=[OB[h]], writes=[rcp])
                S.op("dve", lambda e, h=h: e.tensor_scalar(out=o_n[:, h * 64:(h + 1) * 64], in0=OB[h][:, 0:64], scalar1=rcp[:, h:h + 1], scalar2=None,
                                                          op0=ALU.mult), reads=[OB[h], rcp], writes=[o_n])
            ob = oTb[j % 2]
            for c in range(2):
                S.op("pe", lambda e, c=c: e.transpose(PT[:, c, :], o_n[:, c * 128:(c + 1) * 128], C.ident[:]), reads=[o_n, C.ident], writes=[PT])
                S.op("act", lambda e, c=c, ob=ob: e.copy(out=ob[:, c, :], in_=PT[:, c, :]), reads=[PT], writes=[ob])
            S.dma("sp", och[j % 2], ov[:, :, j * 128:(j + 1) * 128], ob[:], reads=[ob])
        S.barrier()


def build_attn_prog(nj=NJ):
    nc = bass.Bass("TRN2", target_bir_lowering=False)
    T = nj * 128
    hnT = nc.dram_tensor("hnT", [D, T], BF16, kind="ExternalInput").ap()
    wq = nc.dram_tensor("wq", [128, 8, 256], BF16, kind="ExternalInput").ap()
    wqi = nc.dram_tensor("wqi", [128, 8, 128], BF16, kind="ExternalInput").ap()
    wwi = nc.dram_tensor("wwi", [128, 8, 4], BF16, kind="ExternalInput").ap()
    kT = nc.dram_tensor("kT", [256, SEQ], BF16, kind="ExternalInput").ap()
    vf = nc.dram_tensor("vf", [SEQ, 260], BF16, kind="ExternalInput").ap()
    kiT = nc.dram_tensor("kiT", [32, SEQ], BF16, kind="ExternalInput").ap()
    relb = nc.dram_tensor("relb", [32, 4], F32, kind="ExternalInput").ap()
    oh = nc.dram_tensor("oh", [32, 640], F32, kind="ExternalInput").ap()
    vd = nc.dram_tensor("vd", [128, 640], F32, kind="ExternalInput").ap()
    cm = nc.dram_tensor("cm", [2, 128, 256], F32, kind="ExternalInput").ap()
    coef = nc.dram_tensor("coef", [128, 18], F32, kind="ExternalInput").ap()
    base = nc.dram_tensor("base", [128, 512], F32, kind="ExternalInput").ap()
    oT = nc.dram_tensor("oT", [256, T], BF16, kind="ExternalOutput").ap()
    ebd = nc.dram_tensor("ebd", [4 * 128 * 640], F32)
    with ExitStack() as es:
        S = Sched(nc, es)
        C = make_consts(S, nc, es)
        ATTN_IMPL(S, nc, C, hnT, wq, wqi, wwi, kT, vf, kiT, relb, oh, vd, cm, coef, base, oT, ebd, nj)
    return nc


def t5_bucket_np(n):
    n = np.maximum(n, 0)
    large = 16 + (np.log(np.maximum(n, 1).astype(np.float32) / 16) / np.log(128 / 16) * 16).astype(np.int32)
    return np.where(n < 16, n, np.minimum(large, 31))


def attn_consts(r):
    dd = np.arange(640)
    d = dd - 127
    bk = t5_bucket_np(d)
    oh = np.zeros((32, 640), np.float32)
    oh[bk, dd] = 1.0
    vd = np.broadcast_to((d >= 0).astype(np.float32)[None, :], (128, 640)).copy()
    t = np.arange(128)[:, None]
    s = np.arange(256)[None, :]
    ev = np.where((s < 128) & (s <= t), 0.0, NEG).astype(np.float32)
    od = np.where((s < 128) | (s - 128 <= t), 0.0, NEG).astype(np.float32)
    c_ev = np.array([[0, 1, 0], [1, 0, 0], [0, 0, 0]], np.float32)
    c_od = np.array([[0, 0, 1], [0, 1, 0], [1, 0, 0]], np.float32)
    if r == 0:
        cm = np.stack([ev, od]); coef = np.stack([c_ev, c_od])
    else:
        cm = np.stack([od, ev]); coef = np.stack([c_od, c_ev])
    coef = np.broadcast_to(coef.reshape(1, 18), (128, 18)).copy()
    base = np.broadcast_to((-DELTA * np.arange(512, dtype=np.float64)).astype(np.float32)[None, :], (128, 512)).copy()
    return {"oh": oh, "vd": vd, "cm": np.ascontiguousarray(cm), "coef": coef, "base": base}


def local_blocks(r):
    return [2 * j + (r if j % 2 == 0 else 1 - r) for j in range(NJ)]


def pad_v(v):
    o = np.ones((v.shape[0], 4, 65), v.dtype)
    o[:, :, :64] = v.reshape(v.shape[0], 4, 64)
    return o.reshape(v.shape[0], 260)


def phase_mixb(S, nc, C, d, ngrp=NGRP):
    xv = d["x1T"].rearrange("(kc p) t -> p kc t", p=128)
    xov = d["x2T"].rearrange("(kc p) t -> p kc t", p=128)
    hnv = d["hnT"].rearrange("(kc p) t -> p kc t", p=128)
    otv = d["oT"].rearrange("(c p) t -> p c t", p=128)
    with ExitStack() as es:
        def sb(n, shp, dt=F32):
            return S.sb("b_" + n, shp, dt, es)
        x = sb("x", [128, 8, 512]); xch = S.chan()
        hn = sb("hn", [128, 8, 512], BF16); hch = S.chan()
        oT = sb("oT", [128, 2, 512], BF16); otc = S.chan()
        pth = sb("pth", [128, 8, 144]); pch = S.chan()
        hgh = sb("hgh", [128, 8, 160]); hgc = S.chan()
        uT = sb("uT", [128, 2, 512])
        vtm = sb("vtm", [128, 256])
        ss = sb("ss", [128, 1]); rs = sb("rs", [128, 1])
        vnp = [sb("vnp%d" % i, [128, 4, 128], BF16) for i in range(2)]
        gmT = sb("gmT", [128, 2, 512], BF16)
        poolT = sb("poolT", [128, 2, 512], BF16)
        convT = sb("convT", [128, 2, 512], BF16)
        sA = sb("sA", [128, 8, 144]); sB = sb("sB", [128, 8, 144])
        W = sb("W", [128, 8, 128])
        dT = sb("dT", [128, 2, 512], BF16)
        acc = sb("acc", [128, 2, 512])
        sq = [sb("sq%d" % i, [128, 512]) for i in range(2)]
        msb = sb("msb", [128, 512]); m2 = sb("m2", [128, 512]); rstd = sb("rstd", [128, 512]); xc = sb("xc", [128, 512])
        tmpg = sb("tmpg", [128, 128])
        wuv = sb("wuv", [128, 8, 512], BF16)
        wsm = sb("wsm", [128, 4, 128], BF16)
        tril = sb("tril", [128, 128], BF16)
        wbT = sb("wbT", [128, 2, 128])
        vg = sb("vg", [128, 256])
        pwbd = sb("pwbd", [128, 2, 128], BF16)
        vec = sb("vec", [128, 12])
        dw = sb("dw", [128, 2, 31])
        invA = sb("invA", [128, 2, 128])
        postg = sb("postg", [128, 8])
        wbr = sb("wbr", [128, 4, 2, 1024], BF16)
        wgz = [sb("wgz%d" % i, [128, 8, 512], BF16) for i in range(2)]
        wgc = [S.chan() for _ in range(2)]
        wout = sb("wout", [128, 8, 1024], BF16)
        sg = [sb("sg%d" % i, [128, 512]) for i in range(2)]
        tt = [sb("tt%d" % i, [128, 512]) for i in range(2)]
        yacc = sb("yacc", [128, 512])
        yT = sb("yT", [128, 8, 512], BF16)
        hT = sb("hT", [128, 8, 512])
        rstd2 = sb("rstd2", [128, 512])
        tmp = [sb("tmp%d" % i, [128, 512]) for i in range(2)]
        ldc = S.chan()
        PA = [S.ps("b_PA%d" % i, [128, 512], F32, es) for i in range(2)]
        PG = [S.ps("b_PG%d" % i, [128, 512], F32, es) for i in range(2)]
        PB = [S.ps("b_PB%d" % i, [128, 512], F32, es) for i in range(2)]
        PW = S.ps("b_PW", [128, 512], F32, es)
        PS = S.ps("b_PS", [128, 512], F32, es)

        for (t_, k) in ((wuv, "wuv"), (wsm, "wsT"), (tril, "tril"), (wbT, "wbT"), (vg, "vg"), (pwbd, "pwbd"), (vec, "vec"), (dw, "dw"),
                        (invA, "invA"), (postg, "postg"), (wbr, "wbr"), (wout, "wout")):
            S.dma("sp", ldc, t_[:], d[k], writes=[t_])
        S.seal(ldc, [wuv, wsm, tril, wbT, vg, pwbd, vec, dw, invA, postg, wbr, wout])
        S.op("pool", lambda e: e.tensor_tensor(out=wsm[:], in0=wsm[:], in1=tril[:].unsqueeze(1).to_broadcast([128, 4, 128]), op=ALU.mult),
             reads=[wsm, tril], writes=[wsm])
        for i in range(2):
            S.op("pool", lambda e, i=i: e.memset(vnp[i][:], 0.0), writes=[vnp[i]])
        wgn = [0]

        def load_wgz(oc):
            i = wgn[0] % 2
            wgn[0] += 1
            S.dma("act", wgc[i], wgz[i][:], d["wgz"][oc], writes=[wgz[i]])
            return wgz[i]

        pai = [0]

        def nextA():
            p = PA[pai[0] % 2]
            pai[0] += 1
            return p

        for g in range(ngrp):
            tsl = slice(g * 512, (g + 1) * 512)
            S.dma("sp", xch, x[:], xv[:, :, tsl], writes=[x])
            S.dma("sp", hch, hn[:], hnv[:, :, tsl], writes=[hn])
            S.dma("sp", otc, oT[:], otv[:, :, tsl], writes=[oT])
            S.dma("sp", pch, pth[:], d["pth"][:, g * 8:(g + 1) * 8, :], writes=[pth])
            S.dma("sp", hgc, hgh[:], d["hgh"][:, g * 8:(g + 1) * 8, :], writes=[hgh])
            wpend = load_wgz(0)
            for c in range(2):
                P = nextA()
                for kc in range(8):
                    S.op("pe", lambda e, kc=kc, c=c, P=P: e.matmul(P[:], lhsT=wuv[:, kc, c * 128:(c + 1) * 128], rhs=hn[:, kc, :],
                                                                   start=(kc == 0), stop=(kc == 7)), reads=[wuv, hn], writes=[P])
                S.op("act", lambda e, c=c, P=P: e.activation(out=uT[:, c, :], in_=P[:], func=AF.Gelu), reads=[P], writes=[uT])
            for tb in range(4):
                bs = slice(tb * 128, (tb + 1) * 128)
                P = nextA()
                for kc in range(8):
                    S.op("pe", lambda e, kc=kc, bs=bs, P=P: e.matmul(P[:, 0:256], lhsT=hn[:, kc, bs], rhs=wuv[:, kc, 256:512],
                                                                     start=(kc == 0), stop=(kc == 7)), reads=[wuv, hn], writes=[P])
                S.op("act", lambda e, P=P: e.activation(out=vtm[:], in_=P[:, 0:256], func=AF.Gelu), reads=[P], writes=[vtm])
                S.op("act", lambda e: e.activation(out=sq[0][:, 0:256], in_=vtm[:], func=AF.Square, accum_out=ss[:, 0:1]),
                     reads=[vtm], writes=[sq[0], ss])
                S.op("act", lambda e: e.activation(out=rs[:], in_=ss[:], func=AF.Sqrt, bias=C.eps[:, 0:1], scale=1.0 / 256.0),
                     reads=[ss, C.eps], writes=[rs])
                S.op("dve", lambda e: e.reciprocal(out=rs[:], in_=rs[:]), reads=[rs], writes=[rs])
                vp = vnp[tb % 2]
                vpv = vp[:].rearrange("p (a b) c -> p a b c", a=2)
                vtv = vtm[:].rearrange("p (a b c) -> p a b c", a=2, b=2)
                vgv = vg[:].rearrange("p (a b c) -> p a b c", a=2, b=2)
                for gp in range(2):
                    S.op("dve", lambda e, gp=gp, vpv=vpv, vtv=vtv, vgv=vgv: e.scalar_tensor_tensor(
                        out=vpv[:, :, gp, gp * 64:(gp + 1) * 64], in0=vtv[:, :, gp, :], scalar=rs[:, 0:1], in1=vgv[:, :, gp, :],
                        op0=ALU.mult, op1=ALU.mult), reads=[vtm, rs, vg, vp], writes=[vp])
                for c in range(2):
                    P = nextA()
                    for gp in range(2):
                        S.op("pe", lambda e, c=c, gp=gp, P=P, vp=vp: e.matmul(P[:, 0:128], lhsT=vp[:, 2 * c + gp, :], rhs=wsm[:, 2 * c + gp, :],
                                                                             start=(gp == 0), stop=(gp == 1)), reads=[vp, wsm], writes=[P])
                    S.op("dve", lambda e, c=c, P=P: e.tensor_tensor(out=tmpg[:], in0=P[:, 0:128], in1=wbT[:, c, :], op=ALU.add),
                         reads=[P, wbT], writes=[tmpg])
                    S.op("dve", lambda e, c=c, bs=bs: e.tensor_tensor(out=gmT[:, c, bs], in0=tmpg[:], in1=uT[:, c, bs], op=ALU.mult),
                         reads=[tmpg, uT], writes=[gmT])
            def shadd(o, i, sh):
                S.op("pool", lambda e: e.tensor_tensor(out=o[:, :, sh:144], in0=i[:, :, sh:144], in1=i[:, :, 0:144 - sh], op=ALU.add),
                     reads=[i], writes=[o])
            shadd(sA, pth, 1)
            S.op("pool", lambda e: e.tensor_copy(out=W[0:64, 0:4, :], in_=sA[0:64, 0:4, 16:144]), reads=[sA], writes=[W])
            shadd(sB, sA, 2)
            S.op("pool", lambda e: e.tensor_copy(out=W[64:128, 0:4, :], in_=sB[64:128, 0:4, 16:144]), reads=[sB], writes=[W])
            shadd(sA, sB, 4)
            S.op("pool", lambda e: e.tensor_copy(out=W[0:64, 4:8, :], in_=sA[0:64, 4:8, 16:144]), reads=[sA], writes=[W])
            shadd(sB, sA, 8)
            S.op("pool", lambda e: e.tensor_copy(out=W[64:128, 4:8, :], in_=sB[64:128, 4:8, 16:144]), reads=[sB], writes=[W])
            for c in range(2):
                S.op("dve", lambda e, c=c: e.scalar_tensor_tensor(out=dT[:, c, :].rearrange("p (b t) -> p b t", b=4), in0=W[:, 4 * c:4 * c + 4, :],
                                                                  scalar=vec[:, 8 + c:9 + c], in1=pth[:, 4 * c:4 * c + 4, 16:144],
                                                                  op0=ALU.mult, op1=ALU.subtract), reads=[W, vec, pth], writes=[dT])
                if g == 0:
                    S.op("dve", lambda e, c=c: e.tensor_tensor(out=tmpg[:], in0=W[:, 4 * c, :], in1=invA[:, c, :], op=ALU.mult),
                         reads=[W, invA], writes=[tmpg])
                    S.op("dve", lambda e, c=c: e.tensor_tensor(out=dT[:, c, 0:128], in0=tmpg[:], in1=pth[:, 4 * c, 16:144], op=ALU.subtract),
                         reads=[tmpg, pth], writes=[dT])
                P = nextA()
                S.op("pe", lambda e, c=c, P=P: e.matmul(P[:], lhsT=pwbd[:, c, :], rhs=dT[:, c, :], start=True, stop=True), reads=[pwbd, dT], writes=[P])
                S.op("act", lambda e, c=c, P=P: e.mul(out=poolT[:, c, :], in_=P[:], mul=vec[:, c:c + 1]), reads=[P, vec], writes=[poolT])
            for c in range(2):
                av = acc[:, c, :].rearrange("p (b t) -> p b t", b=4)
                S.op("dve", lambda e, c=c, av=av: e.tensor_scalar(out=av, in0=hgh[:, 4 * c:4 * c + 4, 2:130], scalar1=dw[:, c, 0:1], scalar2=vec[:, 2 + c:3 + c],
                                                                 op0=ALU.mult, op1=ALU.add), reads=[hgh, dw, vec], writes=[acc])
                for k in range(1, 31):
                    S.op("dve", lambda e, c=c, k=k, av=av: e.scalar_tensor_tensor(out=av, in0=hgh[:, 4 * c:4 * c + 4, 2 + k:130 + k], scalar=dw[:, c, k:k + 1],
                                                                                 in1=av, op0=ALU.mult, op1=ALU.add), reads=[hgh, dw, acc], writes=[acc])
            for c in range(2):
                S.op("pe", lambda e, c=c: e.matmul(PW[:], lhsT=C.ones256[:], rhs=acc[:, c, :], start=(c == 0), stop=(c == 1)),
                     reads=[acc, C.ones256], writes=[PW])
            for c in range(2):
                S.op("act", lambda e, c=c: e.activation(out=sq[c][:], in_=acc[:, c, :], func=AF.Square), reads=[acc], writes=[sq[c]])
                S.op("pe", lambda e, c=c: e.matmul(PS[:], lhsT=C.ones256[:], rhs=sq[c][:], start=(c == 0), stop=(c == 1)),
                     reads=[sq[c], C.ones256], writes=[PS])
            S.op("act", lambda e: e.copy(out=msb[:], in_=PW[:]), reads=[PW], writes=[msb])
            S.op("dve", lambda e: e.tensor_tensor(out=m2[:], in0=msb[:], in1=msb[:], op=ALU.mult), reads=[msb], writes=[m2])
            S.op("dve", lambda e: e.tensor_tensor(out=m2[:], in0=PS[:], in1=m2[:], op=ALU.subtract), reads=[PS, m2], writes=[m2])
            S.op("act", lambda e: e.activation(out=rstd[:], in_=m2[:], func=AF.Sqrt, bias=C.eps[:, 0:1], scale=1.0), reads=[m2, C.eps], writes=[rstd])
            S.op("dve", lambda e: e.reciprocal(out=rstd[:], in_=rstd[:]), reads=[rstd], writes=[rstd])
            for c in range(2):
                S.op("dve", lambda e, c=c: e.tensor_tensor(out=xc[:], in0=acc[:, c, :], in1=msb[:], op=ALU.subtract), reads=[acc, msb], writes=[xc])
                S.op("dve", lambda e, c=c: e.scalar_tensor_tensor(out=xc[:], in0=xc[:], scalar=vec[:, 4 + c:5 + c], in1=rstd[:], op0=ALU.mult, op1=ALU.mult),
                     reads=[xc, vec, rstd], writes=[xc])
                S.op("act", lambda e, c=c: e.activation(out=convT[:, c, :], in_=xc[:], func=AF.Silu, bias=vec[:, 6 + c:7 + c], scale=1.0),
                     reads=[xc, vec], writes=[convT])
            brs = [gmT, poolT, oT, convT]
            for oc in range(8):
                w = wpend
                if oc + 1 < 8:
                    wpend = load_wgz(oc + 1)
                for n in range(4):
                    G = PG[n % 2]
                    B = PB[n % 2]
                    for kc in range(8):
                        S.op("pe", lambda e, kc=kc, n=n, G=G, w=w: e.matmul(G[:], lhsT=w[:, kc, n * 128:(n + 1) * 128], rhs=hn[:, kc, :],
                                                                           start=(kc == 0), stop=(kc == 7)), reads=[w, hn], writes=[G])
                    for c in range(2):
                        S.op("pe", lambda e, c=c, n=n, B=B, oc=oc: e.matmul(B[:], lhsT=wbr[:, n, c, oc * 128:(oc + 1) * 128], rhs=brs[n][:, c, :],
                                                                           start=(c == 0), stop=(c == 1)), reads=[wbr, brs[n]], writes=[B])
                    s_ = sg[n % 2]
                    S.op("act", lambda e, s_=s_, G=G: e.activation(out=s_[:], in_=G[:], func=AF.Sigmoid), reads=[G], writes=[s_])
                    if n == 0:
                        S.op("dve", lambda e, s_=s_, B=B: e.tensor_tensor(out=yacc[:], in0=s_[:], in1=B[:], op=ALU.mult), reads=[s_, B], writes=[yacc])
                    else:
                        t_ = tt[n % 2]
                        S.op("dve", lambda e, s_=s_, B=B, t_=t_: e.tensor_tensor(out=t_[:], in0=s_[:], in1=B[:], op=ALU.mult), reads=[s_, B], writes=[t_])
                        if n < 3:
                            S.op("pool", lambda e, t_=t_: e.tensor_tensor(out=yacc[:], in0=yacc[:], in1=t_[:], op=ALU.add), reads=[yacc, t_], writes=[yacc])
                        else:
                            S.op("pool", lambda e, t_=t_, oc=oc: e.tensor_tensor(out=yT[:, oc, :], in0=yacc[:], in1=t_[:], op=ALU.add),
                                 reads=[yacc, t_], writes=[yT])
            for oc in range(8):
                for kc in range(8):
                    S.op("pe", lambda e, kc=kc, oc=oc: e.matmul(PW[:], lhsT=wout[:, kc, oc * 128:(oc + 1) * 128], rhs=yT[:, kc, :],
                                                               start=(kc == 0), stop=(kc == 7)), reads=[wout, yT], writes=[PW])
                S.op("act", lambda e, oc=oc: e.copy(out=hT[:, oc, :], in_=PW[:]), reads=[PW], writes=[hT])
                s = sq[oc % 2]
                S.op("act", lambda e, s=s: e.activation(out=s[:], in_=PW[:], func=AF.Square), reads=[PW], writes=[s])
                S.op("pe", lambda e, s=s, oc=oc: e.matmul(PS[:], lhsT=C.onesD[:], rhs=s[:], start=(oc == 0), stop=(oc == 7)),
                     reads=[s, C.onesD], writes=[PS])
            rstd_from_ms(S, PS, rstd2, C)
            for oc in range(8):
                t = tmp[oc % 2]
                S.op("dve", lambda e, t=t, oc=oc: e.scalar_tensor_tensor(out=t[:], in0=hT[:, oc, :], scalar=postg[:, oc:oc + 1], in1=rstd2[:],
                                                                        op0=ALU.mult, op1=ALU.mult), reads=[hT, postg, rstd2], writes=[t])
                S.op("dve", lambda e, t=t, oc=oc: e.tensor_tensor(out=x[:, oc, :], in0=t[:], in1=x[:, oc, :], op=ALU.add), reads=[t, x], writes=[x])
            S.dma("sp", xch, xov[:, :, tsl], x[:], reads=[x])
        S.barrier()


MIXB_IN = {"x1T": ([D, TC], F32), "hnT": ([D, TC], BF16), "oT": ([256, TC], BF16), "pth": ([128, 64, 144], F32), "hgh": ([128, 64, 160], F32),
           "wuv": ([128, 8, 512], BF16), "wsT": ([128, 4, 128], BF16), "tril": ([128, 128], BF16), "wbT": ([128, 2, 128], F32),
           "vg": ([128, 256], F32), "pwbd": ([128, 2, 128], BF16), "vec": ([128, 12], F32), "dw": ([128, 2, 31], F32),
           "invA": ([128, 2, 128], F32), "postg": ([128, 8], F32), "wbr": ([128, 4, 2, 1024], BF16), "wgz": ([8, 128, 8, 512], BF16),
           "wout": ([128, 8, 1024], BF16)}


def build_mixb_prog(ngrp=NGRP):
    nc = bass.Bass("TRN2", target_bir_lowering=False)
    T = ngrp * 512
    d = {}
    for k, (shp, dt) in MIXB_IN.items():
        shp = list(shp)
        if k in ("x1T", "hnT", "oT"):
            shp[1] = T
        if k in ("pth", "hgh"):
            shp[1] = ngrp * 8
        d[k] = nc.dram_tensor(k, shp, dt, kind="ExternalInput").ap()
    d["x2T"] = nc.dram_tensor("x2T", [D, T], F32, kind="ExternalOutput").ap()
    with ExitStack() as es:
        S = Sched(nc, es)
        C = make_consts(S, nc, es)
        phase_mixb(S, nc, C, d, ngrp)
    return nc


def mixb_weights(P, l, wb_in, wb_br, wb_out, wb_pool, wb_ws):
    w = {}
    w["wuv"] = lay_kc(wb_in[:, 0:512])
    w["wsT"] = np.ascontiguousarray(wb_ws.transpose(2, 0, 1))
    s = np.arange(128)
    w["tril"] = (s[:, None] <= s[None, :]).astype(NPBF)
    gb = P["gm_b"][l]
    w["wbT"] = np.ascontiguousarray(np.stack([np.repeat(gb[0:2], 64, axis=0), np.repeat(gb[2:4], 64, axis=0)], axis=1)).astype(np.float32)
    w["vg"] = np.broadcast_to(P["gm_v_g"][l][None, :], (128, 256)).astype(np.float32).copy()
    pw = np.zeros((128, 2, 128), NPBF)
    for g in range(4):
        c, o = g // 2, (g % 2) * 64
        pw[o:o + 64, c, o:o + 64] = wb_pool[g]
    w["pwbd"] = pw
    vec = np.zeros((128, 12), np.float32)
    for c in range(2):
        sl = slice(c * 128, (c + 1) * 128)
        vec[:, c] = P["pool_scale"][l][sl]
        vec[:, 2 + c] = P["conv_b"][l][sl]
        vec[:, 4 + c] = P["conv_ln_g"][l][sl]
        vec[:, 6 + c] = P["conv_ln_b"][l][sl]
    vec[:64, 8], vec[64:, 8], vec[:64, 9], vec[64:, 9] = 1 / 2, 1 / 4, 1 / 8, 1 / 16
    w["vec"] = vec
    w["dw"] = np.ascontiguousarray(P["conv_dw"][l].reshape(31, 2, 128).transpose(2, 1, 0)).astype(np.float32)
    w["postg"] = lay_gain(P["mix_post_g"][l])
    w["wbr"] = np.ascontiguousarray(wb_br.reshape(4, 2, 128, D).transpose(2, 0, 1, 3))
    gz = wb_in[:, O_GZ:].reshape(8, 128, 4, 8, 128)
    w["wgz"] = np.ascontiguousarray(gz.transpose(3, 1, 0, 2, 4).reshape(8, 128, 8, 512))
    w["wout"] = lay_kc(wb_out)
    return w


def inv_first(pos0):
    cnt = pos0 + 1 + np.arange(128, dtype=np.float32)
    o = np.zeros((128, 2, 128), np.float32)
    for g, wdw in enumerate((2, 4, 8, 16)):
        c, p0 = g // 2, (g % 2) * 64
        o[p0:p0 + 64, c, :] = (1.0 / np.minimum(cnt, float(wdw)))[None, :]
    return o


def halo_rows(fullT, blks, halo):
    pad = np.concatenate([np.zeros((256, halo), fullT.dtype), fullT], axis=1)
    ngrp = len(blks) // 4
    o = np.zeros((128, ngrp * 8, halo + 128), np.float32)
    for j, bk in enumerate(blks):
        g, b = j // 4, j % 4
        seg = pad[:, bk * 128:bk * 128 + halo + 128]
        for c in range(2):
            o[:, g * 8 + c * 4 + b, :] = seg[c * 128:(c + 1) * 128]
    return o


WCH = 8192


def build_cast_prog(ncols):
    nc = bass.Bass("TRN2", target_bir_lowering=False)
    src = nc.dram_tensor("src", [128, ncols], F32, kind="ExternalInput").ap()
    dst = nc.dram_tensor("dst", [128, ncols], BF16, kind="ExternalOutput").ap()
    with ExitStack() as es:
        S = Sched(nc, es)
        bufs = [S.sb("w_b%d" % i, [128, WCH], BF16, es) for i in range(2)]
        chs = [S.chan() for _ in range(2)]
        for i in range(ncols // WCH):
            b = bufs[i % 2]
            sl = slice(i * WCH, (i + 1) * WCH)
            S.dma("pool", chs[i % 2], b[:], src[:, sl], writes=[b])
            S.dma("sp", chs[i % 2], dst[:, sl], b[:], reads=[b])
        S.barrier()
    return nc


_PROGS = {}


def _prog(name, fn, *a):
    k = (name,) + a
    if k not in _PROGS:
        _PROGS[k] = fn(*a)
    return _PROGS[k]


def _run(nc, in_maps):
    res = run_bass_kernel_spmd(nc, in_maps, core_ids=list(range(NCORES)))
    import os
    if os.environ.get("KDBGF"):
        for c, r in enumerate(res.results):
            for k, v in r.items():
                a = np.asarray(v).astype(np.float32)
                if not np.isfinite(a).all():
                    bad = np.argwhere(~np.isfinite(a))
                    print("NONFINITE core", c, k, a.shape, "count", len(bad), "first", bad[:3].tolist(), flush=True)
        print("launch done", [k for k in res.results[0].keys()], flush=True)
    return res.results


WNAMES = ["ffn1_w_gu", "ffn1_w_down", "w_in", "gm_ws", "pool_w", "w_branch", "w_out", "ffn2_w_gu", "ffn2_w_down"]


def cast_weights(P):
    flat = np.concatenate([np.ascontiguousarray(P[k], dtype=np.float32).reshape(-1) for k in WNAMES])
    n = flat.size
    per = NCORES * 128 * WCH
    npad = (n + per - 1) // per * per
    buf = np.zeros(npad, np.float32)
    buf[:n] = flat
    ncols = npad // (NCORES * 128)
    shards = buf.reshape(NCORES, 128, ncols)
    nc = _prog("cast", build_cast_prog, ncols)
    res = _run(nc, [{"src": shards[c]} for c in range(NCORES)])
    out = np.concatenate([np.asarray(res[c]["dst"]).reshape(-1) for c in range(NCORES)])[:n]
    W = {}
    o = 0
    for k in WNAMES:
        sz = P[k].size
        W[k] = out[o:o + sz].reshape(P[k].shape)
        o += sz
    return W


def kernel(**P):
    P = {k: np.asarray(v) for k, v in P.items()}
    x = P["x"].astype(np.float32)
    W = cast_weights(P)
    toks = []
    for c in range(NCORES):
        r = c % 2
        toks.append(np.concatenate([np.arange(bk * 128, (bk + 1) * 128) for bk in local_blocks(r)]))
    xT = [np.ascontiguousarray(x[c // 2][toks[c]].T) for c in range(NCORES)]
    nc_f = _prog("ffn", build_ffn_prog, NGRP)
    nc_m1 = _prog("m1", build_m1_prog, NGRP)
    nc_a = _prog("attn", build_attn_prog, NJ)
    nc_b = _prog("mixb", build_mixb_prog, NGRP)
    aconst = [attn_consts(r) for r in range(2)]
    relb = P["rel_bias"].astype(np.float32)

    def ffn(xT, pre, post, wgu, wdn):
        im = {"preg": lay_gain(pre), "postg": lay_gain(post), "wgu": lay_wgu(wgu), "wdn": lay_wdn(wdn)}
        res = _run(nc_f, [dict(im, xT=xT[c]) for c in range(NCORES)])
        return [np.asarray(res[c]["xoT"]) for c in range(NCORES)]

    for l in range(DEPTH):
        xT = ffn(xT, P["ffn1_pre_g"][l], P["ffn1_post_g"][l], W["ffn1_w_gu"][l], W["ffn1_w_down"][l])
        wi = W["w_in"][l]
        w1 = np.concatenate([wi[:, O_K:O_K + 256], wi[:, O_P:O_P + 256], wi[:, O_A:O_A + 256], wi[:, O_GT:O_GT + 256]], axis=1)
        im = {"preg": lay_gain(P["mix_pre_g"][l]), "w1": lay_kc(w1), "wki": lay_kc(wi[:, O_KI:O_KI + 32]), "wv": lay_kc(wi[:, O_VV:O_VV + 256])}
        r1 = _run(nc_m1, [dict(im, xT=xT[c]) for c in range(NCORES)])
        kTf, vff, kif, pTf, hgf = [], [], [], [], []
        for b in range(NB):
            kT_ = np.zeros((256, SEQ), NPBF); vf_ = np.zeros((SEQ, 256), NPBF); ki_ = np.zeros((32, SEQ), NPBF)
            pT_ = np.zeros((256, SEQ), np.float32); hg_ = np.zeros((256, SEQ), np.float32)
            for c in (2 * b, 2 * b + 1):
                kT_[:, toks[c]] = np.asarray(r1[c]["kT"]); vf_[toks[c]] = np.asarray(r1[c]["vtok"]); ki_[:, toks[c]] = np.asarray(r1[c]["kiT"])
                pT_[:, toks[c]] = np.asarray(r1[c]["pT"]); hg_[:, toks[c]] = np.asarray(r1[c]["hgT"])
            kTf.append(kT_); vff.append(pad_v(vf_)); kif.append(ki_); pTf.append(pT_); hgf.append(hg_)
        hnT = [np.asarray(r1[c]["hnT"]) for c in range(NCORES)]
        ima = {"wq": lay_kc(wi[:, O_Q:O_Q + 256]), "wqi": lay_kc(wi[:, O_QI:O_QI + 128]), "wwi": lay_kc(wi[:, O_WI:O_WI + 4]), "relb": relb}
        ra = _run(nc_a, [dict(ima, hnT=hnT[c], kT=kTf[c // 2], vf=vff[c // 2], kiT=kif[c // 2], **aconst[c % 2]) for c in range(NCORES)])
        wm = mixb_weights(P, l, wi, W["w_branch"][l], W["w_out"][l], W["pool_w"][l], W["gm_ws"][l])
        imb = []
        for c in range(NCORES):
            blks = local_blocks(c % 2)
            imb.append(dict(wm, x1T=xT[c], hnT=hnT[c], oT=np.asarray(ra[c]["oT"]), pth=halo_rows(pTf[c // 2], blks, 16),
                            hgh=halo_rows(hgf[c // 2], blks, 32), invA=inv_first(blks[0] * 128)))
        rb = _run(nc_b, imb)
        xT = [np.asarray(rb[c]["x2T"]) for c in range(NCORES)]
        xT = ffn(xT, P["ffn2_pre_g"][l], P["ffn2_post_g"][l], W["ffn2_w_gu"][l], W["ffn2_w_down"][l])
    out = np.zeros((NB, SEQ, D), np.float32)
    for c in range(NCORES):
        out[c // 2][toks[c]] = xT[c].T
    return out


NIT_A = 30


def bis_on_dve(j):
    return False


def phase_attn2(S, nc, C, hnT_d, wq_d, wqi_d, wwi_d, kT_d, vf_d, kiT_d, relb_d, oh_d, vd_d, cm_d, coef_d, base_d,
                oT_d, ebd_t, nj=NJ):
    hnv = hnT_d.rearrange("(kc p) t -> p kc t", p=128)
    ov = oT_d.rearrange("(c p) t -> p c t", p=128)
    with ExitStack() as es:
        def sb(n, shp, dt=F32):
            return S.sb("a2_" + n, shp, dt, es)
        kT = sb("kT", [128, 2, SEQ], BF16)
        vx = sb("vx", [128, 64, 260], BF16)
        ki2 = sb("ki2", [64, SEQ], BF16)
        sacc = [sb("sacc%d" % i, [128, SEQ]) for i in range(2)]
        junkA = sb("junk", [128, SEQ], BF16)
        junkD = junkA
        wq = sb("wq", [128, 8, 256], BF16)
        wqi = sb("wqi", [128, 8, 128], BF16)
        wwi = sb("wwi", [128, 8, 4], BF16)
        cm = sb("cm", [128, 2, 256])
        coef = sb("coef", [128, 18])
        base = sb("base", [128, 512])
        near = [[sb("near%d_%d" % (a, b), [128, 4, 128], BF16) for b in range(3)] for a in range(2)]
        cb = sb("cb", [128, 32])
        zc = sb("zc", [128, 1])
        S.op("pool", lambda e: e.memset(zc[:], 0.0), writes=[zc])
        hnb = [sb("hn%d" % i, [128, 8, 128], BF16) for i in range(2)]
        hch = [S.chan() for _ in range(2)]
        qm = [sb("qm%d" % i, [128, 4, 128], BF16) for i in range(2)]
        qiA = [sb("qiA%d" % i, [64, 128], BF16) for i in range(2)]
        qiB = [sb("qiB%d" % i, [64, 128], BF16) for i in range(2)]
        wt = [sb("wt%d" % i, [128, 4]) for i in range(2)]
        thr = [sb("thr%d" % i, [128, 1]) for i in range(2)]
        nm = [[sb("nm%d_%d" % (i, k), [128, 1]) for k in range(2)] for i in range(2)]
        accs = [sb("accs%d" % i, [128, 1]) for i in range(2)]
        sgs = [sb("sgs%d" % i, [128, 1]) for i in range(2)]
        mid = sb("mid", [128, 1]); cnt = sb("cnt", [128, 1]); stp = sb("stp", [128, 1])
        tmp = [sb("tmp%d" % i, [128, 512]) for i in range(2)]
        mk = [sb("mk%d" % i, [128, 128], BF16) for i in range(2)]
        E = [sb("E%d" % i, [128, 4, 128], BF16) for i in range(2)]
        Pm = [sb("P%d" % i, [128, 4, 128], BF16) for i in range(2)]
        rcp = sb("rcp", [128, 4])
        o_n = sb("on", [128, 256], BF16)
        oTb = [sb("oT%d" % i, [128, 2, 128], BF16) for i in range(2)]
        och = [S.chan() for _ in range(2)]
        ldc = S.chan(); ebc = S.chan(); ndc = S.chan()
        es2 = ExitStack()
        def sb2(n, shp, dt=F32):
            return S.sb("a2t_" + n, shp, dt, es2)
        rb = sb2("rb", [32, 4]); oh = sb2("oh", [32, 640]); vd = sb2("vd", [128, 640])
        rbh = sb2("rbh", [32, 128]); eb = sb2("eb", [128, 640]); negc = sb2("negc", [128, 1])
        nd = [sb2("nd%d" % i, [128, 4, 128]) for i in range(2)]
        ntmp_b = tmp[0]
        SPA = [S.ps("a2_SPA%d" % i, [128, 512], F32, es) for i in range(2)]
        LG = S.ps("a2_LG", [128, 512], F32, es)
        OB = [S.ps("a2_OB%d" % i, [128, 512], F32, es) for i in range(4)]
        PT = S.ps("a2_PT", [128, 2, 128], BF16, es)

        S.dma("sp", ldc, kT[:], kT_d.rearrange("(c p) s -> p c s", p=128), writes=[kT])
        S.dma("sp", ldc, ki2[0:32, :], kiT_d, writes=[ki2])
        S.dma("sp", ldc, ki2[32:64, :], kiT_d, writes=[ki2])
        vfv = vf_d.rearrange("(k p) c -> p k c", p=128)
        for kk in range(8):
            S.dma("sp", ldc, vx[:, kk * 8:(kk + 1) * 8, :], vfv[:, kk * 8:(kk + 1) * 8, :], writes=[vx])
        for (t_, d_) in ((wq, wq_d), (wqi, wqi_d), (wwi, wwi_d), (cm, cm_d.rearrange("a p s -> p a s")), (coef, coef_d), (base, base_d),
                         (rb, relb_d), (oh, oh_d), (vd, vd_d)):
            S.dma("sp", ldc, t_[:], d_, writes=[t_])
        S.seal(ldc, [kT, ki2, vx, wq, wqi, wwi, cm, coef, base, rb, oh, vd])
        for j in range(nj):
            S.op("pool", lambda e, j=j: e.memset(cb[:, j:j + 1], float((2 * j + 2) * 128 - 511)), writes=[cb])
        for i in range(2):
            S.op("pool", lambda e, i=i: e.memset(qm[i][:], 0.0), writes=[qm[i]])

        ebv = RawAP(ebd_t, 0, [[128 * 640, 4], [640, 128], [1, 640]])
        for h in range(4):
            S.op("dve", lambda e, h=h: e.tensor_copy(out=rbh[:], in_=rb[:, h:h + 1].to_broadcast([32, 128])), reads=[rb], writes=[rbh])
            S.op("pe", lambda e: e.matmul(SPA[0][:], lhsT=rbh[:], rhs=oh[:, 0:512], start=True, stop=True), reads=[rbh, oh], writes=[SPA[0]])
            S.op("pe", lambda e: e.matmul(SPA[1][:, 0:128], lhsT=rbh[:], rhs=oh[:, 512:640], start=True, stop=True), reads=[rbh, oh], writes=[SPA[1]])
            S.op("dve", lambda e: e.tensor_scalar(out=negc[:], in0=SPA[1][:, 127:128], scalar1=-1.0, scalar2=None, op0=ALU.mult),
                 reads=[SPA[1]], writes=[negc])
            S.op("act", lambda e: e.activation(out=eb[:, 0:512], in_=SPA[0][:], func=AF.Exp, bias=negc[:, 0:1], scale=1.0),
                 reads=[SPA[0], negc], writes=[eb])
            S.op("act", lambda e: e.activation(out=eb[:, 512:640], in_=SPA[1][:, 0:128], func=AF.Exp, bias=negc[:, 0:1], scale=1.0),
                 reads=[SPA[1], negc], writes=[eb])
            S.op("dve", lambda e: e.tensor_tensor(out=eb[:], in0=eb[:], in1=vd[:], op=ALU.mult), reads=[eb, vd], writes=[eb])
            S.dma("sp", ebc, ebv[h], eb[:], reads=[eb])
            ebuf = Buf("ebd")
            ebuf.w = (ebc, S.cnt[ebc])
            for dl in range(2):
                src = RawAP(ebd_t, h * 128 * 640 + 128 * dl + 127, [[639, 128], [1, 128]])
                S.dma("sp", ndc, nd[dl][:, h, :], src, reads=[ebuf], writes=[nd[dl]])
        S.seal(ndc, nd)
        for a in range(2):
            for b in range(3):
                ci = (a * 3 + b) * 3
                S.op("dve", lambda e, ci=ci: e.tensor_scalar(out=ntmp_b[:].rearrange("p (h t) -> p h t", h=4), in0=nd[0][:], scalar1=coef[:, ci:ci + 1], scalar2=None, op0=ALU.mult),
                     reads=[nd[0], coef], writes=[ntmp_b])
                S.op("dve", lambda e, ci=ci: e.scalar_tensor_tensor(out=ntmp_b[:].rearrange("p (h t) -> p h t", h=4), in0=nd[1][:], scalar=coef[:, ci + 1:ci + 2], in1=ntmp_b[:].rearrange("p (h t) -> p h t", h=4),
                                                                   op0=ALU.mult, op1=ALU.add), reads=[nd[1], coef, ntmp_b], writes=[ntmp_b])
                S.op("dve", lambda e, ci=ci, a=a, b=b: e.tensor_scalar(out=near[a][b][:], in0=ntmp_b[:].rearrange("p (h t) -> p h t", h=4), scalar1=coef[:, ci + 2:ci + 3], scalar2=None, op0=ALU.add),
                     reads=[ntmp_b, coef], writes=[near[a][b]])
        S.barrier()
        es2.close()

        S.dma("sp", hch[0], hnb[0][:], hnv[:, :, 0:128], writes=[hnb[0]])

        class _Rec:
            def __init__(self):
                self.ops = []

            def op(self, *a, **k):
                self.ops.append(lambda: S.op(*a, **k))

            def dma(self, *a, **k):
                self.ops.append(lambda: S.dma(*a, **k))

        def ops_A(j):
            R = _Rec()
            stage_A(j, R)
            return R.ops

        def stage_A(j, S):
            p = j % 2
            L = (2 * j + 2) * 128
            hb = hnb[p]
            if j + 1 < nj:
                S.dma("sp", hch[(j + 1) % 2], hnb[(j + 1) % 2][:], hnv[:, :, (j + 1) * 128:(j + 2) * 128], writes=[hnb[(j + 1) % 2]])
            for c in range(2):
                P = SPA[c]
                for kc in range(8):
                    S.op("pe", lambda e, kc=kc, c=c, P=P: e.matmul(P[:, 0:128], lhsT=wq[:, kc, c * 128:(c + 1) * 128], rhs=hb[:, kc, :],
                                                                   start=(kc == 0), stop=(kc == 7)), reads=[wq, hb], writes=[P])
                S.op("act", lambda e, c=c, P=P: e.copy(out=qm[p][0:64, 2 * c, :], in_=P[0:64, 0:128]), reads=[P], writes=[qm[p]])
                S.op("act", lambda e, c=c, P=P: e.copy(out=qm[p][64:128, 2 * c + 1, :], in_=P[64:128, 0:128]), reads=[P], writes=[qm[p]])
            for c, qi_ in ((0, qiA[p]), (1, qiB[p])):
                P = SPA[c]
                for kc in range(8):
                    S.op("pe", lambda e, kc=kc, c=c, P=P: e.matmul(P[0:64, 128:256], lhsT=wqi[:, kc, c * 64:(c + 1) * 64], rhs=hb[:, kc, :],
                                                                   start=(kc == 0), stop=(kc == 7)), reads=[wqi, hb], writes=[P])
                S.op("act", lambda e, qi_=qi_, P=P: e.copy(out=qi_[:], in_=P[0:64, 128:256]), reads=[P], writes=[qi_])
            P = SPA[0]
            for kc in range(8):
                S.op("pe", lambda e, kc=kc, P=P: e.matmul(P[:, 256:260], lhsT=hb[:, kc, :], rhs=wwi[:, kc, :], start=(kc == 0), stop=(kc == 7)),
                     reads=[wwi, hb], writes=[P])
            S.op("dve", lambda e, P=P: e.tensor_scalar(out=wt[p][:], in0=P[:, 256:260], scalar1=0.5 * (32.0 ** -0.5), scalar2=None, op0=ALU.mult),
                 reads=[P], writes=[wt[p]])
            sa = sacc[p]
            nch = (L + 511) // 512
            for c in range(nch):
                n = min(512, L - c * 512)
                ks = slice(c * 512, c * 512 + n)
                for h in range(4):
                    qi_ = qiA[p] if h < 2 else qiB[p]
                    pb = 32 * (h % 2)
                    SPh = SPA[h % 2]
                    S.op("pe", lambda e, qi_=qi_, pb=pb, ks=ks, n=n, SPh=SPh: e.matmul(SPh[:, 0:n], lhsT=qi_[pb:pb + 32, :], rhs=ki2[pb:pb + 32, ks],
                                                                                     start=True, stop=True), reads=[qi_, ki2], writes=[SPh])
                    t_ = tmp[h % 2]
                    S.op("dve", lambda e, h=h, t_=t_, n=n, SPh=SPh: e.tensor_scalar(out=t_[:, 0:n], in0=SPh[:, 0:n], scalar1=zc[:, 0:1], scalar2=wt[p][:, h:h + 1],
                                                                                   op0=ALU.max, op1=ALU.mult), reads=[SPh, wt[p], zc], writes=[t_])
                    in1 = base[:, 0:n] if h == 0 else sa[:, ks]
                    S.op("dve", lambda e, t_=t_, n=n, ks=ks, in1=in1: e.tensor_tensor(out=sa[:, ks], in0=t_[:, 0:n], in1=in1, op=ALU.add),
                         reads=[t_, base, sa], writes=[sa])
                if c > 0:
                    S.op("dve", lambda e, ks=ks, c=c: e.tensor_scalar(out=sa[:, ks], in0=sa[:, ks], scalar1=-DELTA * 512.0 * c, scalar2=None, op0=ALU.add),
                         reads=[sa], writes=[sa])
            S.op("dve", lambda e: e.tensor_tensor(out=sa[:, L - 256:L], in0=sa[:, L - 256:L], in1=cm[:, p, :], op=ALU.add),
                 reads=[sa, cm], writes=[sa])

        def ops_B(j):
            p = j % 2
            L = (2 * j + 2) * 128
            sa = sacc[p]
            ops = []
            if j == 0:
                ops.append(lambda: S.op("dve", lambda e: e.memset(thr[p][:], -RBIS), writes=[thr[p]]))
            elif bis_on_dve(j):
                lo = thr[p]
                ops.append(lambda: S.op("dve", lambda e: e.memset(lo[:], -RBIS), writes=[lo]))
                for k in range(1, NIT + 1):
                    wk = 2.0 * RBIS * (2.0 ** -k)
                    ops.append(lambda wk=wk: S.op("dve", lambda e: e.tensor_scalar(out=mid[:], in0=lo[:], scalar1=wk, scalar2=None, op0=ALU.add),
                                                  reads=[lo], writes=[mid]))
                    ops.append(lambda: S.op("dve", lambda e: e.tensor_scalar(out=junkD[:, 0:L], in0=sa[:, 0:L], scalar1=mid[:, 0:1], scalar2=0.0,
                                                                            op0=ALU.is_ge, op1=ALU.add, accum_out=cnt[:, 0:1]),
                                            reads=[sa, mid], writes=[junkD, cnt]))
                    ops.append(lambda wk=wk: S.op("dve", lambda e: e.tensor_scalar(out=stp[:], in0=cnt[:], scalar1=255.5, scalar2=wk, op0=ALU.is_ge, op1=ALU.mult),
                                                  reads=[cnt], writes=[stp]))
                    ops.append(lambda: S.op("dve", lambda e: e.tensor_tensor(out=lo[:], in0=lo[:], in1=stp[:], op=ALU.add), reads=[lo, stp], writes=[lo]))
            else:
                ops.append(lambda: S.op("pool", lambda e: e.memset(nm[p][0][:], 0.0), writes=[nm[p][0]]))
                for k in range(1, NIT_A + 1):
                    step = RBIS * (2.0 ** -k)
                    cur, nxt = nm[p][(k - 1) % 2], nm[p][k % 2]
                    ops.append(lambda cur=cur: S.op("act", lambda e: e.activation(out=junkA[:, 0:L], in_=sa[:, 0:L], func=AF.Sign, bias=cur[:, 0:1], scale=1.0,
                                                                                 accum_out=accs[p][:, 0:1]), reads=[sa, cur], writes=[junkA, accs[p]]))
                    ops.append(lambda: S.op("act", lambda e: e.activation(out=sgs[p][:], in_=accs[p][:], func=AF.Sign, bias=cb[:, j:j + 1], scale=1.0),
                                            reads=[accs[p], cb], writes=[sgs[p]]))
                    ops.append(lambda cur=cur, nxt=nxt, step=step: S.op("act", lambda e: e.activation(out=nxt[:], in_=sgs[p][:], func=AF.Identity, bias=cur[:, 0:1],
                                                                                                     scale=-step), reads=[sgs[p], cur], writes=[nxt]))
                fin = nm[p][NIT_A % 2]
                slast = RBIS * (2.0 ** -NIT_A)
                ops.append(lambda: S.op("dve", lambda e: e.tensor_scalar(out=thr[p][:], in0=fin[:], scalar1=-1.0, scalar2=-slast, op0=ALU.mult, op1=ALU.add),
                                        reads=[fin], writes=[thr[p]]))
            return ops

        def ops_D(j):
            p = j % 2
            nk = 2 * j + 2
            sa = sacc[p]
            ops = []

            def s1(kb):
                m_ = mk[kb % 2]
                kb_s = slice(kb * 128, (kb + 1) * 128)
                E_ = E[kb % 2]
                r = []
                r.append(lambda: S.op("dve", lambda e: e.tensor_scalar(out=m_[:], in0=sa[:, kb_s], scalar1=thr[p][:, 0:1], scalar2=None, op0=ALU.is_ge),
                                      reads=[sa, thr[p]], writes=[m_]))
                r.append(lambda: S.op("pe", lambda e: e.transpose(PT[:, kb % 2, :], m_[:], C.ident[:]), reads=[m_, C.ident], writes=[PT]))
                for c in range(2):
                    r.append(lambda c=c: S.op("pe", lambda e: e.matmul(LG[:, c * 256:(c + 1) * 256], lhsT=kT[:, c, kb_s],
                                                                       rhs=qm[p][:, 2 * c:2 * c + 2, :].rearrange("p a t -> p (a t)"),
                                                                       start=True, stop=True), reads=[kT, qm[p]], writes=[LG]))
                r.append(lambda: S.op("act", lambda e: e.activation(out=E_[:].rearrange("p h t -> p (h t)"), in_=LG[:], func=AF.Exp, scale=0.125),
                                      reads=[LG], writes=[E_]))
                return r

            def s2(kb):
                E_ = E[kb % 2]
                P_ = Pm[kb % 2]
                r = []
                r.append(lambda: S.op("dve", lambda e: e.tensor_tensor(out=P_[:], in0=E_[:], in1=PT[:, kb % 2, :].unsqueeze(1).to_broadcast([128, 4, 128]),
                                                                      op=ALU.mult), reads=[E_, PT], writes=[P_]))
                slot = kb - (2 * j - 1)
                if slot >= 0:
                    nr = near[p][slot]
                    r.append(lambda: S.op("dve", lambda e: e.tensor_tensor(out=P_[:], in0=P_[:], in1=nr[:], op=ALU.mult), reads=[P_, nr], writes=[P_]))
                for h in range(4):
                    r.append(lambda h=h: S.op("pe", lambda e: e.matmul(OB[h][:, 0:65], lhsT=P_[:, h, :], rhs=vx[:, kb, h * 65:(h + 1) * 65],
                                                                       start=(kb == 0), stop=(kb == nk - 1)), reads=[P_, vx], writes=[OB[h]]))
                return r

            ops += s1(0)
            for kb in range(nk):
                if kb + 1 < nk:
                    ops += s1(kb + 1)
                ops += s2(kb)
            for h in range(4):
                ops.append(lambda h=h: S.op("dve", lambda e: e.reciprocal(out=rcp[:, h:h + 1], in_=OB[h][:, 64:65]), reads=[OB[h]], writes=[rcp]))
                ops.append(lambda h=h: S.op("dve", lambda e: e.tensor_scalar(out=o_n[:, h * 64:(h + 1) * 64], in0=OB[h][:, 0:64], scalar1=rcp[:, h:h + 1],
                                                                            scalar2=None, op0=ALU.mult), reads=[OB[h], rcp], writes=[o_n]))
            ob = oTb[j % 2]
            for c in range(2):
                ops.append(lambda c=c: S.op("pe", lambda e: e.transpose(PT[:, c, :], o_n[:, c * 128:(c + 1) * 128], C.ident[:]), reads=[o_n, C.ident], writes=[PT]))
                ops.append(lambda c=c: S.op("act", lambda e: e.copy(out=ob[:, c, :], in_=PT[:, c, :]), reads=[PT], writes=[ob]))
            ops.append(lambda: S.dma("sp", och[j % 2], ov[:, :, j * 128:(j + 1) * 128], ob[:], reads=[ob]))
            return ops

        def merge(a, b):
            na, nb = len(a), len(b)
            ia = ib = 0
            while ia < na or ib < nb:
                if ib >= nb or (ia < na and ia * nb <= ib * na):
                    a[ia]()
                    ia += 1
                else:
                    b[ib]()
                    ib += 1

        merge(ops_A(0) + ops_B(0), [])
        for j in range(nj):
            if j + 1 < nj:
                merge(ops_A(j + 1) + ops_B(j + 1), ops_D(j))
            else:
                merge([], ops_D(j))
        S.barrier()


ATTN_IMPL = phase_attn2
```

```python
import numpy as np
import ml_dtypes
from contextlib import ExitStack
import concourse.bass as bass
import concourse.mybir as mybir
from concourse.bass_utils import run_bass_kernel_spmd

F32 = mybir.dt.float32
BF16 = mybir.dt.bfloat16
AF = mybir.ActivationFunctionType
ALU = mybir.AluOpType
AX = mybir.AxisListType
NPBF = ml_dtypes.bfloat16

D = 1024
SEQ = 8192
NB = 4
DEPTH = 4
DFF = 2816
NFC = DFF // 128
IN_COLS = 6308
TC = 4096
NGRP = TC // 512
EPS = 1e-6
NCORES = 8


class Buf:
    def __init__(self, name, t=None):
        self.name = name
        self.t = t
        self.w = None
        self.r = []

    def __getitem__(self, k):
        return self.t[k]


class Sched:
    ENG = ("pe", "act", "dve", "pool", "sp")

    def __init__(self, nc, es):
        self.nc = nc
        self.es = es
        self.e = {"pe": nc.tensor, "act": nc.scalar, "dve": nc.vector, "pool": nc.gpsimd, "sp": nc.sync}
        self.sems = {}
        self.cnt = {}
        self.seen = {e: {} for e in self.ENG}
        for e in self.ENG:
            self.sems[e] = es.enter_context(nc.semaphore("c_" + e))
            self.cnt[e] = 0
        self.nchan = 0
        self.n_ins = 0

    def chan(self):
        k = "ch%d" % self.nchan
        self.nchan += 1
        self.sems[k] = self.es.enter_context(self.nc.semaphore(k))
        self.cnt[k] = 0
        return k

    def sb(self, name, shape, dt, es=None):
        t = (es or self.es).enter_context(self.nc.sbuf_tensor(name, list(shape), dt))
        return Buf(name, t)

    def ps(self, name, shape, dt, es=None):
        t = (es or self.es).enter_context(self.nc.psum_tensor(name, list(shape), dt))
        return Buf(name, t)

    def _waits(self, eng, reads, writes):
        evs = []
        for b in reads:
            if b.w is not None:
                evs.append(b.w)
        for b in writes:
            if b.w is not None:
                evs.append(b.w)
            evs.extend(b.r)
        waits = {}
        for (k, v) in evs:
            if k == "pe" and eng == "pe":
                continue
            if self.seen[eng].get(k, 0) >= v:
                continue
            waits[k] = max(waits.get(k, 0), v)
        for k, v in waits.items():
            self.seen[eng][k] = v
            self.e[eng].wait_ge(self.sems[k], v)

    def _mark(self, ev, reads, writes):
        for b in reads:
            b.r.append(ev)
            if len(b.r) > 12:
                d = {}
                for (k, v) in b.r:
                    d[k] = max(d.get(k, 0), v)
                b.r = list(d.items())
        for b in writes:
            b.w = ev
            b.r = []

    def op(self, eng, fn, reads=(), writes=()):
        self._waits(eng, reads, writes)
        self.cnt[eng] += 1
        ev = (eng, self.cnt[eng])
        fn(self.e[eng]).then_inc(self.sems[eng], 1)
        self.n_ins += 1
        self._mark(ev, reads, writes)
        return ev

    def dma(self, q, ch, out, in_, reads=(), writes=(), **kw):
        self._waits(q, reads, writes)
        self.cnt[ch] += 16
        ev = (ch, self.cnt[ch])
        self.e[q].dma_start(out=out, in_=in_, **kw).then_inc(self.sems[ch], 16)
        self.n_ins += 1
        self._mark(ev, reads, writes)
        return ev

    def seal(self, ch, bufs):
        for b in bufs:
            b.w = (ch, self.cnt[ch])

    def wait_bufs(self, eng, bufs):
        self._waits(eng, [], bufs)

    def barrier(self, bufs=()):
        for eng in self.ENG:
            for k in list(self.cnt.keys()):
                v = self.cnt[k]
                if v == 0 or (k == eng and eng == "pe"):
                    continue
                if self.seen[eng].get(k, 0) >= v:
                    continue
                self.seen[eng][k] = v
                self.e[eng].wait_ge(self.sems[k], v)


class Consts:
    pass


def make_consts(S, nc, es):
    C = Consts()
    C.onesD = S.sb("onesD", [128, 128], F32, es)
    S.op("pool", lambda e: e.memset(C.onesD[:], 1.0 / D), writes=[C.onesD])
    C.ones256 = S.sb("ones256", [128, 128], F32, es)
    S.op("pool", lambda e: e.memset(C.ones256[:], 1.0 / 256.0), writes=[C.ones256])
    C.eps = S.sb("epsc", [128, 1], F32, es)
    S.op("pool", lambda e: e.memset(C.eps[:], EPS), writes=[C.eps])
    C.ident = S.sb("ident", [128, 128], BF16, es)
    S.op("pool", lambda e: e.memset(C.ident[:], 1.0), writes=[C.ident])
    S.op("pool", lambda e: e.affine_select(out=C.ident[:], in_=C.ident[:], pattern=[[-1, 128]],
                                           compare_op=ALU.is_equal, fill=0.0, base=0, channel_multiplier=1),
         reads=[C.ident], writes=[C.ident])
    return C


def rstd_from_ms(S, ms_ps, rstd, C, n=512):
    S.op("act", lambda e: e.activation(out=rstd[:, :n], in_=ms_ps[:, :n], func=AF.Sqrt, bias=C.eps[:, 0:1], scale=1.0),
         reads=[ms_ps, C.eps], writes=[rstd])
    S.op("dve", lambda e: e.reciprocal(out=rstd[:, :n], in_=rstd[:, :n]), reads=[rstd], writes=[rstd])


def phase_ffn(S, nc, C, xT_d, xoT_d, preg_d, postg_d, wgu_d, wdn_d, ngrp=NGRP):
    xv = xT_d.rearrange("(kc p) t -> p kc t", p=128)
    xov = xoT_d.rearrange("(kc p) t -> p kc t", p=128)
    with ExitStack() as es:
        NX = 2
        xs = [S.sb("f_x%d" % i, [128, 8, 512], F32, es) for i in range(NX)]
        xch = [S.chan() for _ in range(NX)]
        xn = S.sb("f_xn", [128, 8, 512], BF16, es)
        sq = [S.sb("f_sq%d" % i, [128, 512], F32, es) for i in range(2)]
        act = S.sb("f_act", [128, NFC, 512], BF16, es)
        sl = [S.sb("f_sl%d" % i, [128, 512], F32, es) for i in range(2)]
        NW = 4
        wgu = [S.sb("f_wgu%d" % i, [128, 8, 256], BF16, es) for i in range(NW)]
        wch = [S.chan() for _ in range(NW)]
        wdn = S.sb("f_wdn", [128, NFC, 1024], BF16, es)
        wdch = S.chan()
        hT = S.sb("f_h", [128, 8, 512], F32, es)
        rstd = S.sb("f_rstd", [128, 512], F32, es)
        rstd2 = S.sb("f_rstd2", [128, 512], F32, es)
        tmp = [S.sb("f_tmp%d" % i, [128, 512], F32, es) for i in range(2)]
        pg = S.sb("f_pg", [128, 8], F32, es)
        qg = S.sb("f_qg", [128, 8], F32, es)
        gch = S.chan()
        och = S.chan()
        pA = [S.ps("f_pA%d" % i, [128, 512], F32, es) for i in range(2)]
        pB = [S.ps("f_pB%d" % i, [128, 512], F32, es) for i in range(2)]
        pO = [S.ps("f_pO%d" % i, [128, 512], F32, es) for i in range(2)]
        pS = S.ps("f_pS", [128, 512], F32, es)
        pS2 = S.ps("f_pS2", [128, 512], F32, es)

        S.dma("sp", gch, pg[:], preg_d, writes=[pg])
        S.dma("sp", gch, qg[:], postg_d, writes=[qg])
        S.seal(gch, [pg, qg])

        def load_x(g):
            b = xs[g % NX]
            S.dma("sp", xch[g % NX], b[:], xv[:, :, g * 512:(g + 1) * 512], writes=[b])

        load_x(0)
        wcount = [0]

        def load_w(fc):
            i = wcount[0] % NW
            wcount[0] += 1
            S.dma("sp", wch[i], wgu[i][:], wgu_d[fc], writes=[wgu[i]])
            return wgu[i]

        for g in range(ngrp):
            x = xs[g % NX]
            if g + 1 < ngrp:
                load_x(g + 1)
            pend = [load_w(0), load_w(1)]
            S.dma("act", wdch, wdn[:], wdn_d, writes=[wdn])
            for kc in range(8):
                s = sq[kc % 2]
                S.op("act", lambda e, s=s, kc=kc: e.activation(out=s[:], in_=x[:, kc, :], func=AF.Square),
                     reads=[x], writes=[s])
                S.op("pe", lambda e, s=s, kc=kc: e.matmul(pS[:], lhsT=C.onesD[:], rhs=s[:], start=(kc == 0), stop=(kc == 7)),
                     reads=[s, C.onesD], writes=[pS])
            rstd_from_ms(S, pS, rstd, C)
            for kc in range(8):
                S.op("dve", lambda e, kc=kc: e.scalar_tensor_tensor(out=xn[:, kc, :], in0=x[:, kc, :], scalar=pg[:, kc:kc + 1],
                                                                    in1=rstd[:], op0=ALU.mult, op1=ALU.mult),
                     reads=[x, pg, rstd], writes=[xn])
            for fc in range(NFC):
                w = pend.pop(0)
                if fc + 2 < NFC:
                    pend.append(load_w(fc + 2))
                A = pA[fc % 2]
                B = pB[fc % 2]
                for kc in range(8):
                    S.op("pe", lambda e, w=w, kc=kc, A=A: e.matmul(A[:], lhsT=w[:, kc, 0:128], rhs=xn[:, kc, :],
                                                                   start=(kc == 0), stop=(kc == 7)),
                         reads=[w, xn], writes=[A])
                for kc in range(8):
                    S.op("pe", lambda e, w=w, kc=kc, B=B: e.matmul(B[:], lhsT=w[:, kc, 128:256], rhs=xn[:, kc, :],
                                                                   start=(kc == 0), stop=(kc == 7)),
                         reads=[w, xn], writes=[B])
                s_ = sl[fc % 2]
                S.op("act", lambda e, s_=s_, A=A: e.activation(out=s_[:], in_=A[:], func=AF.Silu), reads=[A], writes=[s_])
                S.op("dve", lambda e, s_=s_, B=B, fc=fc: e.tensor_tensor(out=act[:, fc, :], in0=s_[:], in1=B[:], op=ALU.mult),
                     reads=[s_, B], writes=[act])
            for oc in range(8):
                O = pO[oc % 2]
                for fc in range(NFC):
                    S.op("pe", lambda e, O=O, fc=fc, oc=oc: e.matmul(O[:], lhsT=wdn[:, fc, oc * 128:(oc + 1) * 128],
                                                                     rhs=act[:, fc, :], start=(fc == 0), stop=(fc == NFC - 1)),
                         reads=[wdn, act], writes=[O])
                S.op("act", lambda e, O=O, oc=oc: e.copy(out=hT[:, oc, :], in_=O[:]), reads=[O], writes=[hT])
                s = sq[oc % 2]
                S.op("act", lambda e, s=s, O=O: e.activation(out=s[:], in_=O[:], func=AF.Square),
                     reads=[O], writes=[s])
                S.op("pe", lambda e, s=s, oc=oc: e.matmul(pS2[:], lhsT=C.onesD[:], rhs=s[:], start=(oc == 0), stop=(oc == 7)),
                     reads=[s, C.onesD], writes=[pS2])
            rstd_from_ms(S, pS2, rstd2, C)
            for oc in range(8):
                t = tmp[oc % 2]
                S.op("dve", lambda e, t=t, oc=oc: e.scalar_tensor_tensor(out=t[:], in0=hT[:, oc, :], scalar=qg[:, oc:oc + 1],
                                                                        in1=rstd2[:], op0=ALU.mult, op1=ALU.mult),
                     reads=[hT, qg, rstd2], writes=[t])
                S.op("dve", lambda e, t=t, oc=oc: e.scalar_tensor_tensor(out=x[:, oc, :], in0=t[:], scalar=0.5,
                                                                         in1=x[:, oc, :], op0=ALU.mult, op1=ALU.add),
                     reads=[t, x], writes=[x])
            S.dma("sp", xch[g % NX], xov[:, :, g * 512:(g + 1) * 512], x[:], reads=[x], writes=[])
        S.barrier()


def build_ffn_prog(ngrp=NGRP):
    nc = bass.Bass("TRN2", target_bir_lowering=False)
    T = ngrp * 512
    xT = nc.dram_tensor("xT", [D, T], F32, kind="ExternalInput").ap()
    preg = nc.dram_tensor("preg", [128, 8], F32, kind="ExternalInput").ap()
    postg = nc.dram_tensor("postg", [128, 8], F32, kind="ExternalInput").ap()
    wgu = nc.dram_tensor("wgu", [NFC, 128, 8, 256], BF16, kind="ExternalInput").ap()
    wdn = nc.dram_tensor("wdn", [128, NFC, 1024], BF16, kind="ExternalInput").ap()
    xoT = nc.dram_tensor("xoT", [D, T], F32, kind="ExternalOutput").ap()
    with ExitStack() as es:
        S = Sched(nc, es)
        C = make_consts(S, nc, es)
        phase_ffn(S, nc, C, xT, xoT, preg, postg, wgu, wdn, ngrp)
    return nc


def lay_gain(g):
    return np.ascontiguousarray(g.reshape(8, 128).T)


def lay_wgu(w):
    g = w[:, :DFF].reshape(8, 128, NFC, 128)
    u = w[:, DFF:].reshape(8, 128, NFC, 128)
    t = np.concatenate([g, u], axis=3)
    return np.ascontiguousarray(t.transpose(2, 1, 0, 3))


def lay_wdn(w):
    return np.ascontiguousarray(w.reshape(NFC, 128, D).transpose(1, 0, 2))


def phase_m1(S, nc, C, xT_d, preg_d, w1_d, wki_d, wv_d, hnT_d, kT_d, vtok_d, kiT_d, pT_d, hgT_d, ngrp=NGRP):
    xv = xT_d.rearrange("(kc p) t -> p kc t", p=128)
    hnv = hnT_d.rearrange("(kc p) t -> p kc t", p=128)
    kv = kT_d.rearrange("(c p) t -> p c t", p=128)
    pv = pT_d.rearrange("(c p) t -> p c t", p=128)
    hgv = hgT_d.rearrange("(c p) t -> p c t", p=128)
    with ExitStack() as es:
        xs = [S.sb("m1_x%d" % i, [128, 8, 512], F32, es) for i in range(2)]
        xch = [S.chan() for _ in range(2)]
        hn = [S.sb("m1_hn%d" % i, [128, 8, 512], BF16, es) for i in range(2)]
        hch = [S.chan() for _ in range(2)]
        sq = [S.sb("m1_sq%d" % i, [128, 512], F32, es) for i in range(2)]
        rstd = S.sb("m1_rstd", [128, 512], F32, es)
        w1 = S.sb("m1_w1", [128, 8, 1024], BF16, es)
        wki = S.sb("m1_wki", [128, 8, 32], BF16, es)
        wv = S.sb("m1_wv", [128, 8, 256], BF16, es)
        pg = S.sb("m1_pg", [128, 8], F32, es)
        wc = S.chan()
        kt = [S.sb("m1_kt%d" % i, [128, 2, 512], BF16, es) for i in range(2)]
        ktch = [S.chan() for _ in range(2)]
        pt = [S.sb("m1_pt%d" % i, [128, 2, 512], F32, es) for i in range(2)]
        ptch = [S.chan() for _ in range(2)]
        hg = [S.sb("m1_hg%d" % i, [128, 2, 512], F32, es) for i in range(2)]
        hgch = [S.chan() for _ in range(2)]
        sg = [S.sb("m1_sg%d" % i, [128, 512], F32, es) for i in range(2)]
        kit = [S.sb("m1_ki%d" % i, [32, 512], BF16, es) for i in range(2)]
        kich = [S.chan() for _ in range(2)]
        vt = [S.sb("m1_vt%d" % i, [128, 4, 256], BF16, es) for i in range(2)]
        vch = [S.chan() for _ in range(2)]
        pS = S.ps("m1_pS", [128, 512], F32, es)
        pP = [S.ps("m1_pP%d" % i, [128, 512], F32, es) for i in range(6)]

        S.dma("sp", wc, pg[:], preg_d, writes=[pg])
        S.dma("sp", wc, w1[:], w1_d, writes=[w1])
        S.dma("sp", wc, wki[:], wki_d, writes=[wki])
        S.dma("sp", wc, wv[:], wv_d, writes=[wv])
        S.seal(wc, [pg, w1, wki, wv])
        S.dma("sp", xch[0], xs[0][:], xv[:, :, 0:512], writes=[xs[0]])
        pi = [0]

        def nextp():
            p = pP[pi[0] % 6]
            pi[0] += 1
            return p

        for g in range(ngrp):
            x = xs[g % 2]
            h = hn[g % 2]
            tsl = slice(g * 512, (g + 1) * 512)
            if g + 1 < ngrp:
                S.dma("sp", xch[(g + 1) % 2], xs[(g + 1) % 2][:], xv[:, :, (g + 1) * 512:(g + 2) * 512], writes=[xs[(g + 1) % 2]])
            for kc in range(8):
                s = sq[kc % 2]
                S.op("act", lambda e, s=s, kc=kc: e.activation(out=s[:], in_=x[:, kc, :], func=AF.Square), reads=[x], writes=[s])
                S.op("pe", lambda e, s=s, kc=kc: e.matmul(pS[:], lhsT=C.onesD[:], rhs=s[:], start=(kc == 0), stop=(kc == 7)),
                     reads=[s, C.onesD], writes=[pS])
            rstd_from_ms(S, pS, rstd, C)
            for kc in range(8):
                S.op("dve", lambda e, kc=kc: e.scalar_tensor_tensor(out=h[:, kc, :], in0=x[:, kc, :], scalar=pg[:, kc:kc + 1],
                                                                    in1=rstd[:], op0=ALU.mult, op1=ALU.mult),
                     reads=[x, pg, rstd], writes=[h])
            S.dma("sp", hch[g % 2], hnv[:, :, tsl], h[:], reads=[h])

            def proj(oc, P):
                for kc in range(8):
                    S.op("pe", lambda e, kc=kc: e.matmul(P[:], lhsT=w1[:, kc, oc * 128:(oc + 1) * 128], rhs=h[:, kc, :],
                                                         start=(kc == 0), stop=(kc == 7)), reads=[w1, h], writes=[P])
            ktb = kt[g % 2]
            for c in range(2):
                P = nextp()
                proj(c, P)
                S.op("act", lambda e, c=c, P=P: e.copy(out=ktb[:, c, :], in_=P[:]), reads=[P], writes=[ktb])
            S.dma("sp", ktch[g % 2], kv[:, :, tsl], ktb[:], reads=[ktb])
            ptb = pt[g % 2]
            for c in range(2):
                P = nextp()
                proj(2 + c, P)
                S.op("dve", lambda e, c=c, P=P: e.tensor_copy(out=ptb[:, c, :], in_=P[:]), reads=[P], writes=[ptb])
            S.dma("sp", ptch[g % 2], pv[:, :, tsl], ptb[:], reads=[ptb])
            hgb = hg[g % 2]
            for c in range(2):
                Pa = nextp()
                proj(4 + c, Pa)
                Pg = nextp()
                proj(6 + c, Pg)
                s_ = sg[c]
                S.op("act", lambda e, s_=s_, Pg=Pg: e.activation(out=s_[:], in_=Pg[:], func=AF.Sigmoid), reads=[Pg], writes=[s_])
                S.op("dve", lambda e, c=c, s_=s_, Pa=Pa: e.tensor_tensor(out=hgb[:, c, :], in0=s_[:], in1=Pa[:], op=ALU.mult),
                     reads=[s_, Pa], writes=[hgb])
            S.dma("sp", hgch[g % 2], hgv[:, :, tsl], hgb[:], reads=[hgb])
            P = nextp()
            for kc in range(8):
                S.op("pe", lambda e, kc=kc: e.matmul(P[0:32, :], lhsT=wki[:, kc, :], rhs=h[:, kc, :], start=(kc == 0), stop=(kc == 7)),
                     reads=[wki, h], writes=[P])
            kib = kit[g % 2]
            S.op("act", lambda e, P=P: e.copy(out=kib[:], in_=P[0:32, :]), reads=[P], writes=[kib])
            S.dma("sp", kich[g % 2], kiT_d[:, tsl], kib[:], reads=[kib])
            vb = vt[g % 2]
            for tb in range(4):
                P = nextp()
                for kc in range(8):
                    S.op("pe", lambda e, kc=kc, tb=tb, P=P: e.matmul(P[:, 0:256], lhsT=h[:, kc, tb * 128:(tb + 1) * 128], rhs=wv[:, kc, :],
                                                                     start=(kc == 0), stop=(kc == 7)), reads=[wv, h], writes=[P])
                S.op("act", lambda e, tb=tb, P=P: e.copy(out=vb[:, tb, :], in_=P[:, 0:256]), reads=[P], writes=[vb])
            S.dma("sp", vch[g % 2], vtok_d[tsl, :].rearrange("(tb p) c -> p tb c", p=128), vb[:], reads=[vb])
        S.barrier()


def build_m1_prog(ngrp=NGRP):
    nc = bass.Bass("TRN2", target_bir_lowering=False)
    T = ngrp * 512
    xT = nc.dram_tensor("xT", [D, T], F32, kind="ExternalInput").ap()
    preg = nc.dram_tensor("preg", [128, 8], F32, kind="ExternalInput").ap()
    w1 = nc.dram_tensor("w1", [128, 8, 1024], BF16, kind="ExternalInput").ap()
    wki = nc.dram_tensor("wki", [128, 8, 32], BF16, kind="ExternalInput").ap()
    wv = nc.dram_tensor("wv", [128, 8, 256], BF16, kind="ExternalInput").ap()
    hnT = nc.dram_tensor("hnT", [D, T], BF16, kind="ExternalOutput").ap()
    kT = nc.dram_tensor("kT", [256, T], BF16, kind="ExternalOutput").ap()
    vtok = nc.dram_tensor("vtok", [T, 256], BF16, kind="ExternalOutput").ap()
    kiT = nc.dram_tensor("kiT", [32, T], BF16, kind="ExternalOutput").ap()
    pT = nc.dram_tensor("pT", [256, T], F32, kind="ExternalOutput").ap()
    hgT = nc.dram_tensor("hgT", [256, T], F32, kind="ExternalOutput").ap()
    with ExitStack() as es:
        S = Sched(nc, es)
        C = make_consts(S, nc, es)
        phase_m1(S, nc, C, xT, preg, w1, wki, wv, hnT, kT, vtok, kiT, pT, hgT, ngrp)
    return nc


def lay_kc(w):
    return np.ascontiguousarray(w.reshape(8, 128, w.shape[1]).transpose(1, 0, 2))


O_U, O_V, O_P, O_Q, O_K, O_VV, O_QI, O_KI, O_WI, O_A, O_GT, O_GZ = 0, 256, 512, 768, 1024, 1280, 1536, 1664, 1696, 1700, 1956, 2212


from concourse.bass_types import AP as RawAP

RBIS = 64.0
NIT = 31
DELTA = 2.0 ** -22
NJ = 32
NEG = -1.0e30


def phase_attn(S, nc, C, hnT_d, wq_d, wqi_d, wwi_d, kT_d, vf_d, kiT_d, relb_d, oh_d, vd_d, cm_d, coef_d, base_d,
               oT_d, ebd_t, nj=NJ):
    hnv = hnT_d.rearrange("(kc p) t -> p kc t", p=128)
    ov = oT_d.rearrange("(c p) t -> p c t", p=128)
    with ExitStack() as es:
        kT = S.sb("a_kT", [128, 2, SEQ], BF16, es)
        vx = S.sb("a_vx", [128, 64, 260], BF16, es)
        ki2 = S.sb("a_ki2", [64, SEQ], BF16, es)
        sacc = S.sb("a_sacc", [128, SEQ], F32, es)
        wq = S.sb("a_wq", [128, 8, 256], BF16, es)
        wqi = S.sb("a_wqi", [128, 8, 128], BF16, es)
        wwi = S.sb("a_wwi", [128, 8, 4], BF16, es)
        cm = S.sb("a_cm", [128, 2, 256], F32, es)
        coef = S.sb("a_coef", [128, 18], F32, es)
        base = S.sb("a_base", [128, 512], F32, es)
        rb = S.sb("a_rb", [32, 4], F32, es)
        oh = S.sb("a_oh", [32, 640], F32, es)
        vd = S.sb("a_vd", [128, 640], F32, es)
        rbh = S.sb("a_rbh", [32, 128], F32, es)
        eb = S.sb("a_eb", [128, 640], F32, es)
        negc = S.sb("a_negc", [128, 1], F32, es)
        nd = [S.sb("a_nd%d" % i, [128, 4, 128], F32, es) for i in range(2)]
        near = [[S.sb("a_near%d_%d" % (a, b), [128, 4, 128], F32, es) for b in range(3)] for a in range(2)]
        hnb = [S.sb("a_hn%d" % i, [128, 8, 128], BF16, es) for i in range(2)]
        hch = [S.chan() for _ in range(2)]
        qm = S.sb("a_qm", [128, 4, 128], BF16, es)
        qiA = S.sb("a_qiA", [64, 128], BF16, es)
        qiB = S.sb("a_qiB", [64, 128], BF16, es)
        wt = S.sb("a_wt", [128, 4], F32, es)
        tmp = [S.sb("a_tmp%d" % i, [128, 512], F32, es) for i in range(2)]
        lo = S.sb("a_lo", [128, 1], F32, es)
        mid = S.sb("a_mid", [128, 1], F32, es)
        cnt = S.sb("a_cnt", [128, 1], F32, es)
        stp = S.sb("a_stp", [128, 1], F32, es)
        junk = S.sb("a_junk", [128, SEQ], BF16, es)
        mk = [S.sb("a_mk%d" % i, [128, 128], BF16, es) for i in range(2)]
        E = [S.sb("a_E%d" % i, [128, 4, 128], BF16, es) for i in range(2)]
        Pm = [S.sb("a_P%d" % i, [128, 4, 128], BF16, es) for i in range(2)]
        rcp = S.sb("a_rcp", [128, 4], F32, es)
        o_n = S.sb("a_on", [128, 256], BF16, es)
        oTb = [S.sb("a_oT%d" % i, [128, 2, 128], BF16, es) for i in range(2)]
        och = [S.chan() for _ in range(2)]
        ldc = S.chan()
        ebc = S.chan()
        ndc = S.chan()
        SP = [S.ps("a_SP%d" % i, [128, 512], F32, es) for i in range(4)]
        PO = S.ps("a_PO", [128, 512], F32, es)
        PT = S.ps("a_PT", [128, 2, 128], BF16, es)
        PJ = [S.ps("a_PJ%d" % i, [128, 512], F32, es) for i in range(2)]
        OB = [PO, SP[2], SP[3], PJ[1]]

        S.dma("sp", ldc, kT[:], kT_d.rearrange("(c p) s -> p c s", p=128), writes=[kT])
        S.dma("sp", ldc, ki2[0:32, :], kiT_d, writes=[ki2])
        S.dma("sp", ldc, ki2[32:64, :], kiT_d, writes=[ki2])
        vfv = vf_d.rearrange("(k p) c -> p k c", p=128)
        for kk in range(8):
            S.dma("sp", ldc, vx[:, kk * 8:(kk + 1) * 8, :], vfv[:, kk * 8:(kk + 1) * 8, :], writes=[vx])
        for (t_, d_) in ((wq, wq_d), (wqi, wqi_d), (wwi, wwi_d), (cm, cm_d.rearrange("a p s -> p a s")), (coef, coef_d), (base, base_d),
                         (rb, relb_d), (oh, oh_d), (vd, vd_d)):
            S.dma("sp", ldc, t_[:], d_, writes=[t_])
        S.seal(ldc, [kT, ki2, vx, wq, wqi, wwi, cm, coef, base, rb, oh, vd])

        ebv = RawAP(ebd_t, 0, [[128 * 640, 4], [640, 128], [1, 640]])
        for h in range(4):
            S.op("dve", lambda e, h=h: e.tensor_copy(out=rbh[:], in_=rb[:, h:h + 1].to_broadcast([32, 128])), reads=[rb], writes=[rbh])
            S.op("pe", lambda e: e.matmul(PJ[0][:], lhsT=rbh[:], rhs=oh[:, 0:512], start=True, stop=True), reads=[rbh, oh], writes=[PJ[0]])
            S.op("pe", lambda e: e.matmul(PJ[1][:, 0:128], lhsT=rbh[:], rhs=oh[:, 512:640], start=True, stop=True), reads=[rbh, oh], writes=[PJ[1]])
            S.op("dve", lambda e: e.tensor_scalar(out=negc[:], in0=PJ[1][:, 127:128], scalar1=-1.0, scalar2=None, op0=ALU.mult),
                 reads=[PJ[1]], writes=[negc])
            S.op("act", lambda e: e.activation(out=eb[:, 0:512], in_=PJ[0][:], func=AF.Exp, bias=negc[:, 0:1], scale=1.0),
                 reads=[PJ[0], negc], writes=[eb])
            S.op("act", lambda e: e.activation(out=eb[:, 512:640], in_=PJ[1][:, 0:128], func=AF.Exp, bias=negc[:, 0:1], scale=1.0),
                 reads=[PJ[1], negc], writes=[eb])
            S.op("dve", lambda e: e.tensor_tensor(out=eb[:], in0=eb[:], in1=vd[:], op=ALU.mult), reads=[eb, vd], writes=[eb])
            S.dma("sp", ebc, ebv[h], eb[:], reads=[eb])
            ebuf = Buf("ebd")
            ebuf.w = (ebc, S.cnt[ebc])
            for dl in range(2):
                src = RawAP(ebd_t, h * 128 * 640 + 128 * dl + 127, [[639, 128], [1, 128]])
                S.dma("sp", ndc, nd[dl][:, h, :], src, reads=[ebuf], writes=[nd[dl]])
        S.seal(ndc, nd)
        for a in range(2):
            for b in range(3):
                t_ = near[a][b]
                ci = (a * 3 + b) * 3
                S.op("dve", lambda e, t_=t_, ci=ci: e.tensor_scalar(out=t_[:], in0=nd[0][:], scalar1=coef[:, ci:ci + 1], scalar2=None, op0=ALU.mult),
                     reads=[nd[0], coef], writes=[t_])
                S.op("dve", lambda e, t_=t_, ci=ci: e.scalar_tensor_tensor(out=t_[:], in0=nd[1][:], scalar=coef[:, ci + 1:ci + 2], in1=t_[:],
                                                                          op0=ALU.mult, op1=ALU.add), reads=[nd[1], coef, t_], writes=[t_])
                S.op("dve", lambda e, t_=t_, ci=ci: e.tensor_scalar(out=t_[:], in0=t_[:], scalar1=coef[:, ci + 2:ci + 3], scalar2=None, op0=ALU.add),
                     reads=[t_, coef], writes=[t_])

        S.op("pool", lambda e: e.memset(qm[:], 0.0), writes=[qm])
        S.dma("sp", hch[0], hnb[0][:], hnv[:, :, 0:128], writes=[hnb[0]])
        import os
        DBG = os.environ.get("KDBG", "")
        for j in range(nj if DBG != "tables" else 0):
            nk = 2 * j + 2
            L = nk * 128
            hb = hnb[j % 2]
            if j + 1 < nj:
                S.dma("sp", hch[(j + 1) % 2], hnb[(j + 1) % 2][:], hnv[:, :, (j + 1) * 128:(j + 2) * 128], writes=[hnb[(j + 1) % 2]])
            for c in range(2):
                P = PJ[c]
                for kc in range(8):
                    S.op("pe", lambda e, kc=kc, c=c, P=P: e.matmul(P[:, 0:128], lhsT=wq[:, kc, c * 128:(c + 1) * 128], rhs=hb[:, kc, :],
                                                                   start=(kc == 0), stop=(kc == 7)), reads=[wq, hb], writes=[P])
                S.op("act", lambda e, c=c, P=P: e.copy(out=qm[0:64, 2 * c, :], in_=P[0:64, 0:128]), reads=[P], writes=[qm])
                S.op("act", lambda e, c=c, P=P: e.copy(out=qm[64:128, 2 * c + 1, :], in_=P[64:128, 0:128]), reads=[P], writes=[qm])
            for c, qi_ in ((0, qiA), (1, qiB)):
                P = PJ[c]
                for kc in range(8):
                    S.op("pe", lambda e, kc=kc, c=c, P=P: e.matmul(P[0:64, 128:256], lhsT=wqi[:, kc, c * 64:(c + 1) * 64], rhs=hb[:, kc, :],
                                                                   start=(kc == 0), stop=(kc == 7)), reads=[wqi, hb], writes=[P])
                S.op("act", lambda e, qi_=qi_, P=P: e.copy(out=qi_[:], in_=P[0:64, 128:256]), reads=[P], writes=[qi_])
            P = PJ[0]
            for kc in range(8):
                S.op("pe", lambda e, kc=kc, P=P: e.matmul(P[:, 256:260], lhsT=hb[:, kc, :], rhs=wwi[:, kc, :], start=(kc == 0), stop=(kc == 7)),
                     reads=[wwi, hb], writes=[P])
            S.op("dve", lambda e, P=P: e.tensor_scalar(out=wt[:], in0=P[:, 256:260], scalar1=0.5 * (32.0 ** -0.5), scalar2=None, op0=ALU.mult),
                 reads=[P], writes=[wt])
            nch = (L + 511) // 512
            for c in range(nch):
                n = min(512, L - c * 512)
                ks = slice(c * 512, c * 512 + n)
                for h in range(4):
                    qi_ = qiA if h < 2 else qiB
                    pb = 32 * (h % 2)
                    S.op("pe", lambda e, h=h, qi_=qi_, pb=pb, ks=ks, n=n: e.matmul(SP[h][:, 0:n], lhsT=qi_[pb:pb + 32, :], rhs=ki2[pb:pb + 32, ks],
                                                                                 start=True, stop=True), reads=[qi_, ki2], writes=[SP[h]])
                    t_ = tmp[h % 2]
                    S.op("act", lambda e, h=h, t_=t_, n=n: e.activation(out=t_[:, 0:n], in_=SP[h][:, 0:n], func=AF.Relu), reads=[SP[h]], writes=[t_])
                    in1 = base[:, 0:n] if h == 0 else sacc[:, ks]
                    S.op("dve", lambda e, h=h, t_=t_, n=n, ks=ks, in1=in1: e.scalar_tensor_tensor(out=sacc[:, ks], in0=t_[:, 0:n], scalar=wt[:, h:h + 1],
                                                                                                 in1=in1, op0=ALU.mult, op1=ALU.add),
                         reads=[t_, wt, base, sacc], writes=[sacc])
                if c > 0:
                    S.op("pool", lambda e, ks=ks, c=c: e.tensor_scalar(out=sacc[:, ks], in0=sacc[:, ks], scalar1=-DELTA * 512.0 * c, scalar2=None, op0=ALU.add),
                         reads=[sacc], writes=[sacc])
            S.op("pool", lambda e, j=j, L=L: e.tensor_tensor(out=sacc[:, L - 256:L], in0=sacc[:, L - 256:L], in1=cm[:, j % 2, :], op=ALU.add),
                 reads=[sacc, cm], writes=[sacc])
            if DBG == "A":
                continue
            S.op("dve", lambda e: e.memset(lo[:], -RBIS), writes=[lo])
            for k in range(1, NIT + 1):
                wk = 2.0 * RBIS * (2.0 ** -k)
                S.op("dve", lambda e, wk=wk: e.tensor_scalar(out=mid[:], in0=lo[:], scalar1=wk, scalar2=None, op0=ALU.add), reads=[lo], writes=[mid])
                S.op("dve", lambda e, L=L: e.tensor_scalar(out=junk[:, 0:L], in0=sacc[:, 0:L], scalar1=mid[:, 0:1], scalar2=0.0, op0=ALU.is_ge, op1=ALU.add,
                                                          accum_out=cnt[:, 0:1]), reads=[sacc, mid], writes=[junk, cnt])
                S.op("dve", lambda e, wk=wk: e.tensor_scalar(out=stp[:], in0=cnt[:], scalar1=255.5, scalar2=wk, op0=ALU.is_ge, op1=ALU.mult),
                     reads=[cnt], writes=[stp])
                S.op("dve", lambda e: e.tensor_tensor(out=lo[:], in0=lo[:], in1=stp[:], op=ALU.add), reads=[lo, stp], writes=[lo])
            if DBG == "B":
                continue
            for kb in range(nk):
                m_ = mk[kb % 2]
                kb_s = slice(kb * 128, (kb + 1) * 128)
                S.op("dve", lambda e, m_=m_, kb_s=kb_s: e.tensor_scalar(out=m_[:], in0=sacc[:, kb_s], scalar1=lo[:, 0:1], scalar2=None, op0=ALU.is_ge),
                     reads=[sacc, lo], writes=[m_])
                S.op("pe", lambda e, m_=m_, kb=kb: e.transpose(PT[:, kb % 2, :], m_[:], C.ident[:]), reads=[m_, C.ident], writes=[PT])
                if DBG == "D1":
                    continue
                Lg = SP[kb % 2]
                for h in range(4):
                    pb = 64 * (h % 2)
                    S.op("pe", lambda e, h=h, pb=pb, kb_s=kb_s, Lg=Lg: e.matmul(Lg[:, h * 128:(h + 1) * 128], lhsT=kT[:, h // 2, kb_s],
                                                                               rhs=qm[:, h, :], start=True, stop=True),
                         reads=[kT, qm], writes=[Lg])
                E_ = E[kb % 2]
                S.op("act", lambda e, E_=E_, Lg=Lg: e.activation(out=E_[:].rearrange("p h t -> p (h t)"), in_=Lg[:], func=AF.Exp, scale=0.125),
                     reads=[Lg], writes=[E_])
                if DBG == "D2":
                    continue
                P_ = Pm[kb % 2]
                S.op("dve", lambda e, E_=E_, P_=P_, kb=kb: e.tensor_tensor(out=P_[:], in0=E_[:], in1=PT[:, kb % 2, :].unsqueeze(1).to_broadcast([128, 4, 128]),
                                                                          op=ALU.mult), reads=[E_, PT], writes=[P_])
                slot = kb - (2 * j - 1)
                if slot >= 0:
                    nr = near[j % 2][slot]
                    S.op("dve", lambda e, P_=P_, nr=nr: e.tensor_tensor(out=P_[:], in0=P_[:], in1=nr[:], op=ALU.mult), reads=[P_, nr], writes=[P_])
                if DBG == "D3":
                    continue
                for h in range(4):
                    S.op("pe", lambda e, h=h, P_=P_, kb=kb, nk=nk: e.matmul(OB[h][:, 0:65], lhsT=P_[:, h, :], rhs=vx[:, kb, h * 65:(h + 1) * 65],
                                                                           start=(kb == 0), stop=(kb == nk - 1)), reads=[P_, vx], writes=[OB[h]])
            if DBG in ("D", "D1", "D2", "D3"):
                continue
            for h in range(4):
                S.op("dve", lambda e, h=h: e.reciprocal(out=rcp[:, h:h + 1], in_=OB[h][:, 64:65]), reads=[OB[h]], writes=[rcp])
                S.op("dve", lambda e, h=h: e.tensor_scalar(out=o_n[:, h * 64:(h + 1) * 64], in0=OB[h][:, 0:64], scalar1=rcp[:, h:h + 1], scalar2=None,
                                                          op0=ALU.mult), reads=[OB[h], rcp], writes=[o_n])
            ob = oTb[j % 2]
            for c in range(2):
                S.op("pe", lambda e, c=c: e.transpose(PT[:, c, :], o_n[:, c * 128:(c + 1) * 128], C.ident[:]), reads=[o_n, C.ident], writes=[PT])
                S.op("act", lambda e, c=c, ob=ob: e.copy(out=ob[:, c, :], in_=PT[:, c, :]), reads=[PT], writes=[ob])
            S.dma("sp", och[j % 2], ov[:, :, j * 128:(j + 1) * 128], ob[:], reads=[ob])
        S.barrier()


def build_attn_prog(nj=NJ):
    nc = bass.Bass("TRN2", target_bir_lowering=False)
    T = nj * 128
    hnT = nc.dram_tensor("hnT", [D, T], BF16, kind="ExternalInput").ap()
    wq = nc.dram_tensor("wq", [128, 8, 256], BF16, kind="ExternalInput").ap()
    wqi = nc.dram_tensor("wqi", [128, 8, 128], BF16, kind="ExternalInput").ap()
    wwi = nc.dram_tensor("wwi", [128, 8, 4], BF16, kind="ExternalInput").ap()
    kT = nc.dram_tensor("kT", [256, SEQ], BF16, kind="ExternalInput").ap()
    vf = nc.dram_tensor("vf", [SEQ, 260], BF16, kind="ExternalInput").ap()
    kiT = nc.dram_tensor("kiT", [32, SEQ], BF16, kind="ExternalInput").ap()
    relb = nc.dram_tensor("relb", [32, 4], F32, kind="ExternalInput").ap()
    oh = nc.dram_tensor("oh", [32, 640], F32, kind="ExternalInput").ap()
    vd = nc.dram_tensor("vd", [128, 640], F32, kind="ExternalInput").ap()
    cm = nc.dram_tensor("cm", [2, 128, 256], F32, kind="ExternalInput").ap()
    coef = nc.dram_tensor("coef", [128, 18], F32, kind="ExternalInput").ap()
    base = nc.dram_tensor("base", [128, 512], F32, kind="ExternalInput").ap()
    oT = nc.dram_tensor("oT", [256, T], BF16, kind="ExternalOutput").ap()
    ebd = nc.dram_tensor("ebd", [4 * 128 * 640], F32)
    with ExitStack() as es:
        S = Sched(nc, es)
        C = make_consts(S, nc, es)
        ATTN_IMPL(S, nc, C, hnT, wq, wqi, wwi, kT, vf, kiT, relb, oh, vd, cm, coef, base, oT, ebd, nj)
    return nc


def t5_bucket_np(n):
    n = np.maximum(n, 0)
    large = 16 + (np.log(np.maximum(n, 1).astype(np.float32) / 16) / np.log(128 / 16) * 16).astype(np.int32)
    return np.where(n < 16, n, np.minimum(large, 31))


def attn_consts(r):
    dd = np.arange(640)
    d = dd - 127
    bk = t5_bucket_np(d)
    oh = np.zeros((32, 640), np.float32)
    oh[bk, dd] = 1.0
    vd = np.broadcast_to((d >= 0).astype(np.float32)[None, :], (128, 640)).copy()
    t = np.arange(128)[:, None]
    s = np.arange(256)[None, :]
    ev = np.where((s < 128) & (s <= t), 0.0, NEG).astype(np.float32)
    od = np.where((s < 128) | (s - 128 <= t), 0.0, NEG).astype(np.float32)
    c_ev = np.array([[0, 1, 0], [1, 0, 0], [0, 0, 0]], np.float32)
    c_od = np.array([[0, 0, 1], [0, 1, 0], [1, 0, 0]], np.float32)
    if r == 0:
        cm = np.stack([ev, od]); coef = np.stack([c_ev, c_od])
    else:
        cm = np.stack([od, ev]); coef = np.stack([c_od, c_ev])
    coef = np.broadcast_to(coef.reshape(1, 18), (128, 18)).copy()
    base = np.broadcast_to((-DELTA * np.arange(512, dtype=np.float64)).astype(np.float32)[None, :], (128, 512)).copy()
    return {"oh": oh, "vd": vd, "cm": np.ascontiguousarray(cm), "coef": coef, "base": base}


def local_blocks(r):
    return [2 * j + (r if j % 2 == 0 else 1 - r) for j in range(NJ)]


def pad_v(v):
    o = np.ones((v.shape[0], 4, 65), v.dtype)
    o[:, :, :64] = v.reshape(v.shape[0], 4, 64)
    return o.reshape(v.shape[0], 260)


def phase_mixb(S, nc, C, d, ngrp=NGRP):
    xv = d["x1T"].rearrange("(kc p) t -> p kc t", p=128)
    xov = d["x2T"].rearrange("(kc p) t -> p kc t", p=128)
    hnv = d["hnT"].rearrange("(kc p) t -> p kc t", p=128)
    otv = d["oT"].rearrange("(c p) t -> p c t", p=128)
    with ExitStack() as es:
        def sb(n, shp, dt=F32):
            return S.sb("b_" + n, shp, dt, es)
        x = sb("x", [128, 8, 512]); xch = S.chan()
        hn = sb("hn", [128, 8, 512], BF16); hch = S.chan()
        oT = sb("oT", [128, 2, 512], BF16); otc = S.chan()
        pth = sb("pth", [128, 8, 144]); pch = S.chan()
        hgh = sb("hgh", [128, 8, 160]); hgc = S.chan()
        uT = sb("uT", [128, 2, 512])
        vtm = sb("vtm", [128, 256])
        ss = sb("ss", [128, 1]); rs = sb("rs", [128, 1])
        vnp = [sb("vnp%d" % i, [128, 4, 128], BF16) for i in range(2)]
        gmT = sb("gmT", [128, 2, 512], BF16)
        poolT = sb("poolT", [128, 2, 512], BF16)
        convT = sb("convT", [128, 2, 512], BF16)
        sA = sb("sA", [128, 8, 144]); sB = sb("sB", [128, 8, 144])
        W = sb("W", [128, 8, 128])
        dT = sb("dT", [128, 2, 512], BF16)
        acc = sb("acc", [128, 2, 512])
        sq = [sb("sq%d" % i, [128, 512]) for i in range(2)]
        msb = sb("msb", [128, 512]); m2 = sb("m2", [128, 512]); rstd = sb("rstd", [128, 512]); xc = sb("xc", [128, 512])
        tmpg = sb("tmpg", [128, 128])
        wuv = sb("wuv", [128, 8, 512], BF16)
        wsm = sb("wsm", [128, 4, 128], BF16)
        tril = sb("tril", [128, 128], BF16)
        wbT = sb("wbT", [128, 2, 128])
        vg = sb("vg", [128, 256])
        pwbd = sb("pwbd", [128, 2, 128], BF16)
        vec = sb("vec", [128, 12])
        dw = sb("dw", [128, 2, 31])
        invA = sb("invA", [128, 2, 128])
        postg = sb("postg", [128, 8])
        wbr = sb("wbr", [128, 4, 2, 1024], BF16)
        wgz = [sb("wgz%d" % i, [128, 8, 512], BF16) for i in range(2)]
        wgc = [S.chan() for _ in range(2)]
        wout = sb("wout", [128, 8, 1024], BF16)
        sg = [sb("sg%d" % i, [128, 512]) for i in range(2)]
        tt = [sb("tt%d" % i, [128, 512]) for i in range(2)]
        yacc = sb("yacc", [128, 512])
        yT = sb("yT", [128, 8, 512], BF16)
        hT = sb("hT", [128, 8, 512])
        rstd2 = sb("rstd2", [128, 512])
        tmp = [sb("tmp%d" % i, [128, 512]) for i in range(2)]
        ldc = S.chan()
        PA = [S.ps("b_PA%d" % i, [128, 512], F32, es) for i in range(2)]
        PG = [S.ps("b_PG%d" % i, [128, 512], F32, es) for i in range(2)]
        PB = [S.ps("b_PB%d" % i, [128, 512], F32, es) for i in range(2)]
        PW = S.ps("b_PW", [128, 512], F32, es)
        PS = S.ps("b_PS", [128, 512], F32, es)

        for (t_, k) in ((wuv, "wuv"), (wsm, "wsT"), (tril, "tril"), (wbT, "wbT"), (vg, "vg"), (pwbd, "pwbd"), (vec, "vec"), (dw, "dw"),
                        (invA, "invA"), (postg, "postg"), (wbr, "wbr"), (wout, "wout")):
            S.dma("sp", ldc, t_[:], d[k], writes=[t_])
        S.seal(ldc, [wuv, wsm, tril, wbT, vg, pwbd, vec, dw, invA, postg, wbr, wout])
        S.op("pool", lambda e: e.tensor_tensor(out=wsm[:], in0=wsm[:], in1=tril[:].unsqueeze(1).to_broadcast([128, 4, 128]), op=ALU.mult),
             reads=[wsm, tril], writes=[wsm])
        for i in range(2):
            S.op("pool", lambda e, i=i: e.memset(vnp[i][:], 0.0), writes=[vnp[i]])
        wgn = [0]

        def load_wgz(oc):
            i = wgn[0] % 2
            wgn[0] += 1
            S.dma("act", wgc[i], wgz[i][:], d["wgz"][oc], writes=[wgz[i]])
            return wgz[i]

        pai = [0]

        def nextA():
            p = PA[pai[0] % 2]
            pai[0] += 1
            return p

        for g in range(ngrp):
            tsl = slice(g * 512, (g + 1) * 512)
            S.dma("sp", xch, x[:], xv[:, :, tsl], writes=[x])
            S.dma("sp", hch, hn[:], hnv[:, :, tsl], writes=[hn])
            S.dma("sp", otc, oT[:], otv[:, :, tsl], writes=[oT])
            S.dma("sp", pch, pth[:], d["pth"][:, g * 8:(g + 1) * 8, :], writes=[pth])
            S.dma("sp", hgc, hgh[:], d["hgh"][:, g * 8:(g + 1) * 8, :], writes=[hgh])
            wpend = load_wgz(0)
            for c in range(2):
                P = nextA()
                for kc in range(8):
                    S.op("pe", lambda e, kc=kc, c=c, P=P: e.matmul(P[:], lhsT=wuv[:, kc, c * 128:(c + 1) * 128], rhs=hn[:, kc, :],
                                                                   start=(kc == 0), stop=(kc == 7)), reads=[wuv, hn], writes=[P])
                S.op("act", lambda e, c=c, P=P: e.activation(out=uT[:, c, :], in_=P[:], func=AF.Gelu), reads=[P], writes=[uT])
            for tb in range(4):
                bs = slice(tb * 128, (tb + 1) * 128)
                P = nextA()
                for kc in range(8):
                    S.op("pe", lambda e, kc=kc, bs=bs, P=P: e.matmul(P[:, 0:256], lhsT=hn[:, kc, bs], rhs=wuv[:, kc, 256:512],
                                                                     start=(kc == 0), stop=(kc == 7)), reads=[wuv, hn], writes=[P])
                S.op("act", lambda e, P=P: e.activation(out=vtm[:], in_=P[:, 0:256], func=AF.Gelu), reads=[P], writes=[vtm])
                S.op("act", lambda e: e.activation(out=sq[0][:, 0:256], in_=vtm[:], func=AF.Square, accum_out=ss[:, 0:1]),
                     reads=[vtm], writes=[sq[0], ss])
                S.op("act", lambda e: e.activation(out=rs[:], in_=ss[:], func=AF.Sqrt, bias=C.eps[:, 0:1], scale=1.0 / 256.0),
                     reads=[ss, C.eps], writes=[rs])
                S.op("dve", lambda e: e.reciprocal(out=rs[:], in_=rs[:]), reads=[rs], writes=[rs])
                vp = vnp[tb % 2]
                vpv = vp[:].rearrange("p (a b) c -> p a b c", a=2)
                vtv = vtm[:].rearrange("p (a b c) -> p a b c", a=2, b=2)
                vgv = vg[:].rearrange("p (a b c) -> p a b c", a=2, b=2)
                for gp in range(2):
                    S.op("dve", lambda e, gp=gp, vpv=vpv, vtv=vtv, vgv=vgv: e.scalar_tensor_tensor(
                        out=vpv[:, :, gp, gp * 64:(gp + 1) * 64], in0=vtv[:, :, gp, :], scalar=rs[:, 0:1], in1=vgv[:, :, gp, :],
                        op0=ALU.mult, op1=ALU.mult), reads=[vtm, rs, vg, vp], writes=[vp])
                for c in range(2):
                    P = nextA()
                    for gp in range(2):
                        S.op("pe", lambda e, c=c, gp=gp, P=P, vp=vp: e.matmul(P[:, 0:128], lhsT=vp[:, 2 * c + gp, :], rhs=wsm[:, 2 * c + gp, :],
                                                                             start=(gp == 0), stop=(gp == 1)), reads=[vp, wsm], writes=[P])
                    S.op("dve", lambda e, c=c, P=P: e.tensor_tensor(out=tmpg[:], in0=P[:, 0:128], in1=wbT[:, c, :], op=ALU.add),
                         reads=[P, wbT], writes=[tmpg])
                    S.op("dve", lambda e, c=c, bs=bs: e.tensor_tensor(out=gmT[:, c, bs], in0=tmpg[:], in1=uT[:, c, bs], op=ALU.mult),
                         reads=[tmpg, uT], writes=[gmT])
            def shadd(o, i, sh):
                S.op("pool", lambda e: e.tensor_tensor(out=o[:, :, sh:144], in0=i[:, :, sh:144], in1=i[:, :, 0:144 - sh], op=ALU.add),
                     reads=[i], writes=[o])
            shadd(sA, pth, 1)
            S.op("pool", lambda e: e.tensor_copy(out=W[0:64, 0:4, :], in_=sA[0:64, 0:4, 16:144]), reads=[sA], writes=[W])
            shadd(sB, sA, 2)
            S.op("pool", lambda e: e.tensor_copy(out=W[64:128, 0:4, :], in_=sB[64:128, 0:4, 16:144]), reads=[sB], writes=[W])
            shadd(sA, sB, 4)
            S.op("pool", lambda e: e.tensor_copy(out=W[0:64, 4:8, :], in_=sA[0:64, 4:8, 16:144]), reads=[sA], writes=[W])
            shadd(sB, sA, 8)
            S.op("pool", lambda e: e.tensor_copy(out=W[64:128, 4:8, :], in_=sB[64:128, 4:8, 16:144]), reads=[sB], writes=[W])
            for c in range(2):
                S.op("dve", lambda e, c=c: e.scalar_tensor_tensor(out=dT[:, c, :].rearrange("p (b t) -> p b t", b=4), in0=W[:, 4 * c:4 * c + 4, :],
                                                                  scalar=vec[:, 8 + c:9 + c], in1=pth[:, 4 * c:4 * c + 4, 16:144],
                                                                  op0=ALU.mult, op1=ALU.subtract), reads=[W, vec, pth], writes=[dT])
                if g == 0:
                    S.op("dve", lambda e, c=c: e.tensor_tensor(out=tmpg[:], in0=W[:, 4 * c, :], in1=invA[:, c, :], op=ALU.mult),
                         reads=[W, invA], writes=[tmpg])
                    S.op("dve", lambda e, c=c: e.tensor_tensor(out=dT[:, c, 0:128], in0=tmpg[:], in1=pth[:, 4 * c, 16:144], op=ALU.subtract),
                         reads=[tmpg, pth], writes=[dT])
                P = nextA()
                S.op("pe", lambda e, c=c, P=P: e.matmul(P[:], lhsT=pwbd[:, c, :], rhs=dT[:, c, :], start=True, stop=True), reads=[pwbd, dT], writes=[P])
                S.op("act", lambda e, c=c, P=P: e.mul(out=poolT[:, c, :], in_=P[:], mul=vec[:, c:c + 1]), reads=[P, vec], writes=[poolT])
            for c in range(2):
                av = acc[:, c, :].rearrange("p (b t) -> p b t", b=4)
                S.op("dve", lambda e, c=c, av=av: e.tensor_scalar(out=av, in0=hgh[:, 4 * c:4 * c + 4, 2:130], scalar1=dw[:, c, 0:1], scalar2=vec[:, 2 + c:3 + c],
                                                                 op0=ALU.mult, op1=ALU.add), reads=[hgh, dw, vec], writes=[acc])
                for k in range(1, 31):
                    S.op("dve", lambda e, c=c, k=k, av=av: e.scalar_tensor_tensor(out=av, in0=hgh[:, 4 * c:4 * c + 4, 2 + k:130 + k], scalar=dw[:, c, k:k + 1],
                                                                                 in1=av, op0=ALU.mult, op1=ALU.add), reads=[hgh, dw, acc], writes=[acc])
            for c in range(2):
                S.op("pe", lambda e, c=c: e.matmul(PW[:], lhsT=C.ones256[:], rhs=acc[:, c, :], start=(c == 0), stop=(c == 1)),
                     reads=[acc, C.ones256], writes=[PW])
            for c in range(2):
                S.op("act", lambda e, c=c: e.activation(out=sq[c][:], in_=acc[:, c, :], func=AF.Square), reads=[acc], writes=[sq[c]])
                S.op("pe", lambda e, c=c: e.matmul(PS[:], lhsT=C.ones256[:], rhs=sq[c][:], start=(c == 0), stop=(c == 1)),
                     reads=[sq[c], C.ones256], writes=[PS])
            S.op("act", lambda e: e.copy(out=msb[:], in_=PW[:]), reads=[PW], writes=[msb])
            S.op("dve", lambda e: e.tensor_tensor(out=m2[:], in0=msb[:], in1=msb[:], op=ALU.mult), reads=[msb], writes=[m2])
            S.op("dve", lambda e: e.tensor_tensor(out=m2[:], in0=PS[:], in1=m2[:], op=ALU.subtract), reads=[PS, m2], writes=[m2])
            S.op("act", lambda e: e.activation(out=rstd[:], in_=m2[:], func=AF.Sqrt, bias=C.eps[:, 0:1], scale=1.0), reads=[m2, C.eps], writes=[rstd])
            S.op("dve", lambda e: e.reciprocal(out=rstd[:], in_=rstd[:]), reads=[rstd], writes=[rstd])
            for c in range(2):
                S.op("dve", lambda e, c=c: e.tensor_tensor(out=xc[:], in0=acc[:, c, :], in1=msb[:], op=ALU.subtract), reads=[acc, msb], writes=[xc])
                S.op("dve", lambda e, c=c: e.scalar_tensor_tensor(out=xc[:], in0=xc[:], scalar=vec[:, 4 + c:5 + c], in1=rstd[:], op0=ALU.mult, op1=ALU.mult),
                     reads=[xc, vec, rstd], writes=[xc])
                S.op("act", lambda e, c=c: e.activation(out=convT[:, c, :], in_=xc[:], func=AF.Silu, bias=vec[:, 6 + c:7 + c], scale=1.0),
                     reads=[xc, vec], writes=[convT])
            brs = [gmT, poolT, oT, convT]
            for oc in range(8):
                w = wpend
                if oc + 1 < 8:
                    wpend = load_wgz(oc + 1)
                for n in range(4):
                    G = PG[n % 2]
                    B = PB[n % 2]
                    for kc in range(8):
                        S.op("pe", lambda e, kc=kc, n=n, G=G, w=w: e.matmul(G[:], lhsT=w[:, kc, n * 128:(n + 1) * 128], rhs=hn[:, kc, :],
                                                                           start=(kc == 0), stop=(kc == 7)), reads=[w, hn], writes=[G])
                    for c in range(2):
                        S.op("pe", lambda e, c=c, n=n, B=B, oc=oc: e.matmul(B[:], lhsT=wbr[:, n, c, oc * 128:(oc + 1) * 128], rhs=brs[n][:, c, :],
                                                                           start=(c == 0), stop=(c == 1)), reads=[wbr, brs[n]], writes=[B])
                    s_ = sg[n % 2]
                    S.op("act", lambda e, s_=s_, G=G: e.activation(out=s_[:], in_=G[:], func=AF.Sigmoid), reads=[G], writes=[s_])
                    if n == 0:
                        S.op("dve", lambda e, s_=s_, B=B: e.tensor_tensor(out=yacc[:], in0=s_[:], in1=B[:], op=ALU.mult), reads=[s_, B], writes=[yacc])
                    else:
                        t_ = tt[n % 2]
                        S.op("dve", lambda e, s_=s_, B=B, t_=t_: e.tensor_tensor(out=t_[:], in0=s_[:], in1=B[:], op=ALU.mult), reads=[s_, B], writes=[t_])
                        if n < 3:
                            S.op("pool", lambda e, t_=t_: e.tensor_tensor(out=yacc[:], in0=yacc[:], in1=t_[:], op=ALU.add), reads=[yacc, t_], writes=[yacc])
                        else:
                            S.op("pool", lambda e, t_=t_, oc=oc: e.tensor_tensor(out=yT[:, oc, :], in0=yacc[:], in1=t_[:], op=ALU.add),
                                 reads=[yacc, t_], writes=[yT])
            for oc in range(8):
                for kc in range(8):
                    S.op("pe", lambda e, kc=kc, oc=oc: e.matmul(PW[:], lhsT=wout[:, kc, oc * 128:(oc + 1) * 128], rhs=yT[:, kc, :],
                                                               start=(kc == 0), stop=(kc == 7)), reads=[wout, yT], writes=[PW])
                S.op("act", lambda e, oc=oc: e.copy(out=hT[:, oc, :], in_=PW[:]), reads=[PW], writes=[hT])
                s = sq[oc % 2]
                S.op("act", lambda e, s=s: e.activation(out=s[:], in_=PW[:], func=AF.Square), reads=[PW], writes=[s])
                S.op("pe", lambda e, s=s, oc=oc: e.matmul(PS[:], lhsT=C.onesD[:], rhs=s[:], start=(oc == 0), stop=(oc == 7)),
                     reads=[s, C.onesD], writes=[PS])
            rstd_from_ms(S, PS, rstd2, C)
            for oc in range(8):
                t = tmp[oc % 2]
                S.op("dve", lambda e, t=t, oc=oc: e.scalar_tensor_tensor(out=t[:], in0=hT[:, oc, :], scalar=postg[:, oc:oc + 1], in1=rstd2[:],
                                                                        op0=ALU.mult, op1=ALU.mult), reads=[hT, postg, rstd2], writes=[t])
                S.op("dve", lambda e, t=t, oc=oc: e.tensor_tensor(out=x[:, oc, :], in0=t[:], in1=x[:, oc, :], op=ALU.add), reads=[t, x], writes=[x])
            S.dma("sp", xch, xov[:, :, tsl], x[:], reads=[x])
        S.barrier()


MIXB_IN = {"x1T": ([D, TC], F32), "hnT": ([D, TC], BF16), "oT": ([256, TC], BF16), "pth": ([128, 64, 144], F32), "hgh": ([128, 64, 160], F32),
           "wuv": ([128, 8, 512], BF16), "wsT": ([128, 4, 128], BF16), "tril": ([128, 128], BF16), "wbT": ([128, 2, 128], F32),
           "vg": ([128, 256], F32), "pwbd": ([128, 2, 128], BF16), "vec": ([128, 12], F32), "dw": ([128, 2, 31], F32),
           "invA": ([128, 2, 128], F32), "postg": ([128, 8], F32), "wbr": ([128, 4, 2, 1024], BF16), "wgz": ([8, 128, 8, 512], BF16),
           "wout": ([128, 8, 1024], BF16)}


def build_mixb_prog(ngrp=NGRP):
    nc = bass.Bass("TRN2", target_bir_lowering=False)
    T = ngrp * 512
    d = {}
    for k, (shp, dt) in MIXB_IN.items():
        shp = list(shp)
        if k in ("x1T", "hnT", "oT"):
            shp[1] = T
        if k in ("pth", "hgh"):
            shp[1] = ngrp * 8
        d[k] = nc.dram_tensor(k, shp, dt, kind="ExternalInput").ap()
    d["x2T"] = nc.dram_tensor("x2T", [D, T], F32, kind="ExternalOutput").ap()
    with ExitStack() as es:
        S = Sched(nc, es)
        C = make_consts(S, nc, es)
        phase_mixb(S, nc, C, d, ngrp)
    return nc


def mixb_weights(P, l, wb_in, wb_br, wb_out, wb_pool, wb_ws):
    w = {}
    w["wuv"] = lay_kc(wb_in[:, 0:512])
    w["wsT"] = np.ascontiguousarray(wb_ws.transpose(2, 0, 1))
    s = np.arange(128)
    w["tril"] = (s[:, None] <= s[None, :]).astype(NPBF)
    gb = P["gm_b"][l]
    w["wbT"] = np.ascontiguousarray(np.stack([np.repeat(gb[0:2], 64, axis=0), np.repeat(gb[2:4], 64, axis=0)], axis=1)).astype(np.float32)
    w["vg"] = np.broadcast_to(P["gm_v_g"][l][None, :], (128, 256)).astype(np.float32).copy()
    pw = np.zeros((128, 2, 128), NPBF)
    for g in range(4):
        c, o = g // 2, (g % 2) * 64
        pw[o:o + 64, c, o:o + 64] = wb_pool[g]
    w["pwbd"] = pw
    vec = np.zeros((128, 12), np.float32)
    for c in range(2):
        sl = slice(c * 128, (c + 1) * 128)
        vec[:, c] = P["pool_scale"][l][sl]
        vec[:, 2 + c] = P["conv_b"][l][sl]
        vec[:, 4 + c] = P["conv_ln_g"][l][sl]
        vec[:, 6 + c] = P["conv_ln_b"][l][sl]
    vec[:64, 8], vec[64:, 8], vec[:64, 9], vec[64:, 9] = 1 / 2, 1 / 4, 1 / 8, 1 / 16
    w["vec"] = vec
    w["dw"] = np.ascontiguousarray(P["conv_dw"][l].reshape(31, 2, 128).transpose(2, 1, 0)).astype(np.float32)
    w["postg"] = lay_gain(P["mix_post_g"][l])
    w["wbr"] = np.ascontiguousarray(wb_br.reshape(4, 2, 128, D).transpose(2, 0, 1, 3))
    gz = wb_in[:, O_GZ:].reshape(8, 128, 4, 8, 128)
    w["wgz"] = np.ascontiguousarray(gz.transpose(3, 1, 0, 2, 4).reshape(8, 128, 8, 512))
    w["wout"] = lay_kc(wb_out)
    return w


def inv_first(pos0):
    cnt = pos0 + 1 + np.arange(128, dtype=np.float32)
    o = np.zeros((128, 2, 128), np.float32)
    for g, wdw in enumerate((2, 4, 8, 16)):
        c, p0 = g // 2, (g % 2) * 64
        o[p0:p0 + 64, c, :] = (1.0 / np.minimum(cnt, float(wdw)))[None, :]
    return o


def halo_rows(fullT, blks, halo):
    pad = np.concatenate([np.zeros((256, halo), fullT.dtype), fullT], axis=1)
    ngrp = len(blks) // 4
    o = np.zeros((128, ngrp * 8, halo + 128), np.float32)
    for j, bk in enumerate(blks):
        g, b = j // 4, j % 4
        seg = pad[:, bk * 128:bk * 128 + halo + 128]
        for c in range(2):
            o[:, g * 8 + c * 4 + b, :] = seg[c * 128:(c + 1) * 128]
    return o


WCH = 8192


def build_cast_prog(ncols):
    nc = bass.Bass("TRN2", target_bir_lowering=False)
    src = nc.dram_tensor("src", [128, ncols], F32, kind="ExternalInput").ap()
    dst = nc.dram_tensor("dst", [128, ncols], BF16, kind="ExternalOutput").ap()
    with ExitStack() as es:
        S = Sched(nc, es)
        bufs = [S.sb("w_b%d" % i, [128, WCH], BF16, es) for i in range(2)]
        chl = [S.chan() for _ in range(2)]
        chs = [S.chan() for _ in range(2)]
        for i in range(ncols // WCH):
            b = bufs[i % 2]
            sl = slice(i * WCH, (i + 1) * WCH)
            S.dma("pool", chl[i % 2], b[:], src[:, sl], writes=[b])
            S.dma("sp", chs[i % 2], dst[:, sl], b[:], reads=[b])
        S.barrier()
    return nc


_PROGS = {}


def _prog(name, fn, *a):
    k = (name,) + a
    if k not in _PROGS:
        _PROGS[k] = fn(*a)
    return _PROGS[k]


def _run(nc, in_maps):
    res = run_bass_kernel_spmd(nc, in_maps, core_ids=list(range(NCORES)))
    import os
    if os.environ.get("KDBGF"):
        for c, r in enumerate(res.results):
            for k, v in r.items():
                a = np.asarray(v).astype(np.float32)
                if not np.isfinite(a).all():
                    bad = np.argwhere(~np.isfinite(a))
                    print("NONFINITE core", c, k, a.shape, "count", len(bad), "first", bad[:3].tolist(), flush=True)
        print("launch done", [k for k in res.results[0].keys()], flush=True)
    return res.results


WNAMES = ["ffn1_w_gu", "ffn1_w_down", "w_in", "gm_ws", "pool_w", "w_branch", "w_out", "ffn2_w_gu", "ffn2_w_down"]


def cast_weights(P):
    flat = np.concatenate([np.ascontiguousarray(P[k], dtype=np.float32).reshape(-1) for k in WNAMES])
    n = flat.size
    per = NCORES * 128 * WCH
    npad = (n + per - 1) // per * per
    buf = np.zeros(npad, np.float32)
    buf[:n] = flat
    ncols = npad // (NCORES * 128)
    shards = buf.reshape(NCORES, 128, ncols)
    nc = _prog("cast", build_cast_prog, ncols)
    res = _run(nc, [{"src": shards[c]} for c in range(NCORES)])
    out = np.concatenate([np.asarray(res[c]["dst"]).reshape(-1) for c in range(NCORES)])[:n]
    W = {}
    o = 0
    for k in WNAMES:
        sz = P[k].size
        W[k] = out[o:o + sz].reshape(P[k].shape)
        o += sz
    return W


def kernel(**P):
    P = {k: np.asarray(v) for k, v in P.items()}
    x = P["x"].astype(np.float32)
    W = cast_weights(P)
    toks = []
    for c in range(NCORES):
        r = c % 2
        toks.append(np.concatenate([np.arange(bk * 128, (bk + 1) * 128) for bk in local_blocks(r)]))
    xT = [np.ascontiguousarray(x[c // 2][toks[c]].T) for c in range(NCORES)]
    nc_f = _prog("ffn", build_ffn_prog, NGRP)
    nc_m1 = _prog("m1", build_m1_prog, NGRP)
    nc_a = _prog("attn", build_attn_prog, NJ)
    nc_b = _prog("mixb", build_mixb_prog, NGRP)
    aconst = [attn_consts(r) for r in range(2)]
    relb = P["rel_bias"].astype(np.float32)

    def ffn(xT, pre, post, wgu, wdn):
        im = {"preg": lay_gain(pre), "postg": lay_gain(post), "wgu": lay_wgu(wgu), "wdn": lay_wdn(wdn)}
        res = _run(nc_f, [dict(im, xT=xT[c]) for c in range(NCORES)])
        return [np.asarray(res[c]["xoT"]) for c in range(NCORES)]

    for l in range(DEPTH):
        xT = ffn(xT, P["ffn1_pre_g"][l], P["ffn1_post_g"][l], W["ffn1_w_gu"][l], W["ffn1_w_down"][l])
        wi = W["w_in"][l]
        w1 = np.concatenate([wi[:, O_K:O_K + 256], wi[:, O_P:O_P + 256], wi[:, O_A:O_A + 256], wi[:, O_GT:O_GT + 256]], axis=1)
        im = {"preg": lay_gain(P["mix_pre_g"][l]), "w1": lay_kc(w1), "wki": lay_kc(wi[:, O_KI:O_KI + 32]), "wv": lay_kc(wi[:, O_VV:O_VV + 256])}
        r1 = _run(nc_m1, [dict(im, xT=xT[c]) for c in range(NCORES)])
        kTf, vff, kif, pTf, hgf = [], [], [], [], []
        for b in range(NB):
            kT_ = np.zeros((256, SEQ), NPBF); vf_ = np.zeros((SEQ, 256), NPBF); ki_ = np.zeros((32, SEQ), NPBF)
            pT_ = np.zeros((256, SEQ), np.float32); hg_ = np.zeros((256, SEQ), np.float32)
            for c in (2 * b, 2 * b + 1):
                kT_[:, toks[c]] = np.asarray(r1[c]["kT"]); vf_[toks[c]] = np.asarray(r1[c]["vtok"]); ki_[:, toks[c]] = np.asarray(r1[c]["kiT"])
                pT_[:, toks[c]] = np.asarray(r1[c]["pT"]); hg_[:, toks[c]] = np.asarray(r1[c]["hgT"])
            kTf.append(kT_); vff.append(pad_v(vf_)); kif.append(ki_); pTf.append(pT_); hgf.append(hg_)
        hnT = [np.asarray(r1[c]["hnT"]) for c in range(NCORES)]
        ima = {"wq": lay_kc(wi[:, O_Q:O_Q + 256]), "wqi": lay_kc(wi[:, O_QI:O_QI + 128]), "wwi": lay_kc(wi[:, O_WI:O_WI + 4]), "relb": relb}
        ra = _run(nc_a, [dict(ima, hnT=hnT[c], kT=kTf[c // 2], vf=vff[c // 2], kiT=kif[c // 2], **aconst[c % 2]) for c in range(NCORES)])
        wm = mixb_weights(P, l, wi, W["w_branch"][l], W["w_out"][l], W["pool_w"][l], W["gm_ws"][l])
        imb = []
        for c in range(NCORES):
            blks = local_blocks(c % 2)
            imb.append(dict(wm, x1T=xT[c], hnT=hnT[c], oT=np.asarray(ra[c]["oT"]), pth=halo_rows(pTf[c // 2], blks, 16),
                            hgh=halo_rows(hgf[c // 2], blks, 32), invA=inv_first(blks[0] * 128)))
        rb = _run(nc_b, imb)
        xT = [np.asarray(rb[c]["x2T"]) for c in range(NCORES)]
        xT = ffn(xT, P["ffn2_pre_g"][l], P["ffn2_post_g"][l], W["ffn2_w_gu"][l], W["ffn2_w_down"][l])
    out = np.zeros((NB, SEQ, D), np.float32)
    for c in range(NCORES):
        out[c // 2][toks[c]] = xT[c].T
    return out


NIT_A = 30


def bis_on_dve(j):
    return False


def phase_attn2(S, nc, C, hnT_d, wq_d, wqi_d, wwi_d, kT_d, vf_d, kiT_d, relb_d, oh_d, vd_d, cm_d, coef_d, base_d,
                oT_d, ebd_t, nj=NJ):
    hnv = hnT_d.rearrange("(kc p) t -> p kc t", p=128)
    ov = oT_d.rearrange("(c p) t -> p c t", p=128)
    with ExitStack() as es:
        def sb(n, shp, dt=F32):
            return S.sb("a2_" + n, shp, dt, es)
        kT = sb("kT", [128, 2, SEQ], BF16)
        vx = sb("vx", [128, 64, 260], BF16)
        ki2 = sb("ki2", [64, SEQ], BF16)
        sacc = [sb("sacc%d" % i, [128, SEQ]) for i in range(2)]
        junkA = sb("junk", [128, SEQ], BF16)
        junkD = junkA
        wq = sb("wq", [128, 8, 256], BF16)
        wqi = sb("wqi", [128, 8, 128], BF16)
        wwi = sb("wwi", [128, 8, 4], BF16)
        cm = sb("cm", [128, 2, 256])
        coef = sb("coef", [128, 18])
        base = sb("base", [128, 512])
        near = [[sb("near%d_%d" % (a, b), [128, 4, 128], BF16) for b in range(3)] for a in range(2)]
        cb = sb("cb", [128, 32])
        zc = sb("zc", [128, 1])
        S.op("pool", lambda e: e.memset(zc[:], 0.0), writes=[zc])
        hnb = [sb("hn%d" % i, [128, 8, 128], BF16) for i in range(2)]
        hch = [S.chan() for _ in range(2)]
        qm = [sb("qm%d" % i, [128, 4, 128], BF16) for i in range(2)]
        qiA = [sb("qiA%d" % i, [64, 128], BF16) for i in range(2)]
        qiB = [sb("qiB%d" % i, [64, 128], BF16) for i in range(2)]
        wt = [sb("wt%d" % i, [128, 4]) for i in range(2)]
        thr = [sb("thr%d" % i, [128, 1]) for i in range(2)]
        nm = [[sb("nm%d_%d" % (i, k), [128, 1]) for k in range(2)] for i in range(2)]
        accs = [sb("accs%d" % i, [128, 1]) for i in range(2)]
        sgs = [sb("sgs%d" % i, [128, 1]) for i in range(2)]
        mid = sb("mid", [128, 1]); cnt = sb("cnt", [128, 1]); stp = sb("stp", [128, 1])
        tmp = [sb("tmp%d" % i, [128, 512]) for i in range(2)]
        mk = [sb("mk%d" % i, [128, 128], BF16) for i in range(2)]
        E = [sb("E%d" % i, [128, 4, 128], BF16) for i in range(2)]
        Pm = [sb("P%d" % i, [128, 4, 128], BF16) for i in range(2)]
        rcp = sb("rcp", [128, 4])
        o_n = sb("on", [128, 256], BF16)
        oTb = [sb("oT%d" % i, [128, 2, 128], BF16) for i in range(2)]
        och = [S.chan() for _ in range(2)]
        ldc = S.chan(); ebc = S.chan(); ndc = S.chan()
        es2 = ExitStack()
        def sb2(n, shp, dt=F32):
            return S.sb("a2t_" + n, shp, dt, es2)
        rb = sb2("rb", [32, 4]); oh = sb2("oh", [32, 640]); vd = sb2("vd", [128, 640])
        rbh = sb2("rbh", [32, 128]); eb = sb2("eb", [128, 640]); negc = sb2("negc", [128, 1])
        nd = [sb2("nd%d" % i, [128, 4, 128]) for i in range(2)]
        ntmp_b = tmp[0]
        SPA = [S.ps("a2_SPA%d" % i, [128, 512], F32, es) for i in range(2)]
        LG = S.ps("a2_LG", [128, 512], F32, es)
        OB = [S.ps("a2_OB%d" % i, [128, 512], F32, es) for i in range(4)]
        PT = S.ps("a2_PT", [128, 2, 128], BF16, es)

        S.dma("sp", ldc, kT[:], kT_d.rearrange("(c p) s -> p c s", p=128), writes=[kT])
        S.dma("sp", ldc, ki2[0:32, :], kiT_d, writes=[ki2])
        S.dma("sp", ldc, ki2[32:64, :], kiT_d, writes=[ki2])
        vfv = vf_d.rearrange("(k p) c -> p k c", p=128)
        for kk in range(8):
            S.dma("sp", ldc, vx[:, kk * 8:(kk + 1) * 8, :], vfv[:, kk * 8:(kk + 1) * 8, :], writes=[vx])
        for (t_, d_) in ((wq, wq_d), (wqi, wqi_d), (wwi, wwi_d), (cm, cm_d.rearrange("a p s -> p a s")), (coef, coef_d), (base, base_d),
                         (rb, relb_d), (oh, oh_d), (vd, vd_d)):
            S.dma("sp", ldc, t_[:], d_, writes=[t_])
        S.seal(ldc, [kT, ki2, vx, wq, wqi, wwi, cm, coef, base, rb, oh, vd])
        for j in range(nj):
            S.op("pool", lambda e, j=j: e.memset(cb[:, j:j + 1], float((2 * j + 2) * 128 - 511)), writes=[cb])
        for i in range(2):
            S.op("pool", lambda e, i=i: e.memset(qm[i][:], 0.0), writes=[qm[i]])

        ebv = RawAP(ebd_t, 0, [[128 * 640, 4], [640, 128], [1, 640]])
        for h in range(4):
            S.op("dve", lambda e, h=h: e.tensor_copy(out=rbh[:], in_=rb[:, h:h + 1].to_broadcast([32, 128])), reads=[rb], writes=[rbh])
            S.op("pe", lambda e: e.matmul(SPA[0][:], lhsT=rbh[:], rhs=oh[:, 0:512], start=True, stop=True), reads=[rbh, oh], writes=[SPA[0]])
            S.op("pe", lambda e: e.matmul(SPA[1][:, 0:128], lhsT=rbh[:], rhs=oh[:, 512:640], start=True, stop=True), reads=[rbh, oh], writes=[SPA[1]])
            S.op("dve", lambda e: e.tensor_scalar(out=negc[:], in0=SPA[1][:, 127:128], scalar1=-1.0, scalar2=None, op0=ALU.mult),
                 reads=[SPA[1]], writes=[negc])
            S.op("act", lambda e: e.activation(out=eb[:, 0:512], in_=SPA[0][:], func=AF.Exp, bias=negc[:, 0:1], scale=1.0),
                 reads=[SPA[0], negc], writes=[eb])
            S.op("act", lambda e: e.activation(out=eb[:, 512:640], in_=SPA[1][:, 0:128], func=AF.Exp, bias=negc[:, 0:1], scale=1.0),
                 reads=[SPA[1], negc], writes=[eb])
            S.op("dve", lambda e: e.tensor_tensor(out=eb[:], in0=eb[:], in1=vd[:], op=ALU.mult), reads=[eb, vd], writes=[eb])
            S.dma("sp", ebc, ebv[h], eb[:], reads=[eb])
            ebuf = Buf("ebd")
            ebuf.w = (ebc, S.cnt[ebc])
            for dl in range(2):
                src = RawAP(ebd_t, h * 128 * 640 + 128 * dl + 127, [[639, 128], [1, 128]])
                S.dma("sp", ndc, nd[dl][:, h, :], src, reads=[ebuf], writes=[nd[dl]])
        S.seal(ndc, nd)
        for a in range(2):
            for b in range(3):
                ci = (a * 3 + b) * 3
                S.op("dve", lambda e, ci=ci: e.tensor_scalar(out=ntmp_b[:].rearrange("p (h t) -> p h t", h=4), in0=nd[0][:], scalar1=coef[:, ci:ci + 1], scalar2=None, op0=ALU.mult),
                     reads=[nd[0], coef], writes=[ntmp_b])
                S.op("dve", lambda e, ci=ci: e.scalar_tensor_tensor(out=ntmp_b[:].rearrange("p (h t) -> p h t", h=4), in0=nd[1][:], scalar=coef[:, ci + 1:ci + 2], in1=ntmp_b[:].rearrange("p (h t) -> p h t", h=4),
                                                                   op0=ALU.mult, op1=ALU.add), reads=[nd[1], coef, ntmp_b], writes=[ntmp_b])
                S.op("dve", lambda e, ci=ci, a=a, b=b: e.tensor_scalar(out=near[a][b][:], in0=ntmp_b[:].rearrange("p (h t) -> p h t", h=4), scalar1=coef[:, ci + 2:ci + 3], scalar2=None, op0=ALU.add),
                     reads=[ntmp_b, coef], writes=[near[a][b]])
        S.barrier()
        es2.close()

        S.dma("sp", hch[0], hnb[0][:], hnv[:, :, 0:128], writes=[hnb[0]])

        class _Rec:
            def __init__(self):
                self.ops = []

            def op(self, *a, **k):
                self.ops.append(lambda: S.op(*a, **k))

            def dma(self, *a, **k):
                self.ops.append(lambda: S.dma(*a, **k))

        def ops_A(j):
            R = _Rec()
            stage_A(j, R)
            return R.ops

        def stage_A(j, S):
            p = j % 2
            L = (2 * j + 2) * 128
            hb = hnb[p]
            if j + 1 < nj:
                S.dma("sp", hch[(j + 1) % 2], hnb[(j + 1) % 2][:], hnv[:, :, (j + 1) * 128:(j + 2) * 128], writes=[hnb[(j + 1) % 2]])
            for c in range(2):
                P = SPA[c]
                for kc in range(8):
                    S.op("pe", lambda e, kc=kc, c=c, P=P: e.matmul(P[:, 0:128], lhsT=wq[:, kc, c * 128:(c + 1) * 128], rhs=hb[:, kc, :],
                                                                   start=(kc == 0), stop=(kc == 7)), reads=[wq, hb], writes=[P])
                S.op("act", lambda e, c=c, P=P: e.copy(out=qm[p][0:64, 2 * c, :], in_=P[0:64, 0:128]), reads=[P], writes=[qm[p]])
                S.op("act", lambda e, c=c, P=P: e.copy(out=qm[p][64:128, 2 * c + 1, :], in_=P[64:128, 0:128]), reads=[P], writes=[qm[p]])
            for c, qi_ in ((0, qiA[p]), (1, qiB[p])):
                P = SPA[c]
                for kc in range(8):
                    S.op("pe", lambda e, kc=kc, c=c, P=P: e.matmul(P[0:64, 128:256], lhsT=wqi[:, kc, c * 64:(c + 1) * 64], rhs=hb[:, kc, :],
                                                                   start=(kc == 0), stop=(kc == 7)), reads=[wqi, hb], writes=[P])
                S.op("act", lambda e, qi_=qi_, P=P: e.copy(out=qi_[:], in_=P[0:64, 128:256]), reads=[P], writes=[qi_])
            P = SPA[0]
            for kc in range(8):
                S.op("pe", lambda e, kc=kc, P=P: e.matmul(P[:, 256:260], lhsT=hb[:, kc, :], rhs=wwi[:, kc, :], start=(kc == 0), stop=(kc == 7)),
                     reads=[wwi, hb], writes=[P])
            S.op("dve", lambda e, P=P: e.tensor_scalar(out=wt[p][:], in0=P[:, 256:260], scalar1=0.5 * (32.0 ** -0.5), scalar2=None, op0=ALU.mult),
                 reads=[P], writes=[wt[p]])
            sa = sacc[p]
            nch = (L + 511) // 512
            for c in range(nch):
                n = min(512, L - c * 512)
                ks = slice(c * 512, c * 512 + n)
                for h in range(4):
                    qi_ = qiA[p] if h < 2 else qiB[p]
                    pb = 32 * (h % 2)
                    SPh = SPA[h % 2]
                    S.op("pe", lambda e, qi_=qi_, pb=pb, ks=ks, n=n, SPh=SPh: e.matmul(SPh[:, 0:n], lhsT=qi_[pb:pb + 32, :], rhs=ki2[pb:pb + 32, ks],
                                                                                     start=True, stop=True), reads=[qi_, ki2], writes=[SPh])
                    t_ = tmp[h % 2]
                    S.op("dve", lambda e, h=h, t_=t_, n=n, SPh=SPh: e.tensor_scalar(out=t_[:, 0:n], in0=SPh[:, 0:n], scalar1=zc[:, 0:1], scalar2=wt[p][:, h:h + 1],
                                                                                   op0=ALU.max, op1=ALU.mult), reads=[SPh, wt[p], zc], writes=[t_])
                    in1 = base[:, 0:n] if h == 0 else sa[:, ks]
                    S.op("dve", lambda e, t_=t_, n=n, ks=ks, in1=in1: e.tensor_tensor(out=sa[:, ks], in0=t_[:, 0:n], in1=in1, op=ALU.add),
                         reads=[t_, base, sa], writes=[sa])
                if c > 0:
                    S.op("dve", lambda e, ks=ks, c=c: e.tensor_scalar(out=sa[:, ks], in0=sa[:, ks], scalar1=-DELTA * 512.0 * c, scalar2=None, op0=ALU.add),
                         reads=[sa], writes=[sa])
            S.op("dve", lambda e: e.tensor_tensor(out=sa[:, L - 256:L], in0=sa[:, L - 256:L], in1=cm[:, p, :], op=ALU.add),
                 reads=[sa, cm], writes=[sa])

        def ops_B(j):
            p = j % 2
            L = (2 * j + 2) * 128
            sa = sacc[p]
            ops = []
            if j == 0:
                ops.append(lambda: S.op("dve", lambda e: e.memset(thr[p][:], -RBIS), writes=[thr[p]]))
            elif bis_on_dve(j):
                lo = thr[p]
                ops.append(lambda: S.op("dve", lambda e: e.memset(lo[:], -RBIS), writes=[lo]))
                for k in range(1, NIT + 1):
                    wk = 2.0 * RBIS * (2.0 ** -k)
                    ops.append(lambda wk=wk: S.op("dve", lambda e: e.tensor_scalar(out=mid[:], in0=lo[:], scalar1=wk, scalar2=None, op0=ALU.add),
                                                  reads=[lo], writes=[mid]))
                    ops.append(lambda: S.op("dve", lambda e: e.tensor_scalar(out=junkD[:, 0:L], in0=sa[:, 0:L], scalar1=mid[:, 0:1], scalar2=0.0,
                                                                            op0=ALU.is_ge, op1=ALU.add, accum_out=cnt[:, 0:1]),
                                            reads=[sa, mid], writes=[junkD, cnt]))
                    ops.append(lambda wk=wk: S.op("dve", lambda e: e.tensor_scalar(out=stp[:], in0=cnt[:], scalar1=255.5, scalar2=wk, op0=ALU.is_ge, op1=ALU.mult),
                                                  reads=[cnt], writes=[stp]))
                    ops.append(lambda: S.op("dve", lambda e: e.tensor_tensor(out=lo[:], in0=lo[:], in1=stp[:], op=ALU.add), reads=[lo, stp], writes=[lo]))
            else:
                ops.append(lambda: S.op("pool", lambda e: e.memset(nm[p][0][:], 0.0), writes=[nm[p][0]]))
                for k in range(1, NIT_A + 1):
                    step = RBIS * (2.0 ** -k)
                    cur, nxt = nm[p][(k - 1) % 2], nm[p][k % 2]
                    ops.append(lambda cur=cur: S.op("act", lambda e: e.activation(out=junkA[:, 0:L], in_=sa[:, 0:L], func=AF.Sign, bias=cur[:, 0:1], scale=1.0,
                                                                                 accum_out=accs[p][:, 0:1]), reads=[sa, cur], writes=[junkA, accs[p]]))
                    ops.append(lambda: S.op("act", lambda e: e.activation(out=sgs[p][:], in_=accs[p][:], func=AF.Sign, bias=cb[:, j:j + 1], scale=1.0),
                                            reads=[accs[p], cb], writes=[sgs[p]]))
                    ops.append(lambda cur=cur, nxt=nxt, step=step: S.op("act", lambda e: e.activation(out=nxt[:], in_=sgs[p][:], func=AF.Identity, bias=cur[:, 0:1],
                                                                                                     scale=-step), reads=[sgs[p], cur], writes=[nxt]))
                fin = nm[p][NIT_A % 2]
                slast = RBIS * (2.0 ** -NIT_A)
                ops.append(lambda: S.op("dve", lambda e: e.tensor_scalar(out=thr[p][:], in0=fin[:], scalar1=-1.0, scalar2=-slast, op0=ALU.mult, op1=ALU.add),
                                        reads=[fin], writes=[thr[p]]))
            return ops

        def ops_D(j):
            p = j % 2
            nk = 2 * j + 2
            sa = sacc[p]
            ops = []

            def s1(kb):
                m_ = mk[kb % 2]
                kb_s = slice(kb * 128, (kb + 1) * 128)
                E_ = E[kb % 2]
                r = []
                r.append(lambda: S.op("dve", lambda e: e.tensor_scalar(out=m_[:], in0=sa[:, kb_s], scalar1=thr[p][:, 0:1], scalar2=None, op0=ALU.is_ge),
                                      reads=[sa, thr[p]], writes=[m_]))
                r.append(lambda: S.op("pe", lambda e: e.transpose(PT[:, kb % 2, :], m_[:], C.ident[:]), reads=[m_, C.ident], writes=[PT]))
                for c in range(2):
                    r.append(lambda c=c: S.op("pe", lambda e: e.matmul(LG[:, c * 256:(c + 1) * 256], lhsT=kT[:, c, kb_s],
                                                                       rhs=qm[p][:, 2 * c:2 * c + 2, :].rearrange("p a t -> p (a t)"),
                                                                       start=True, stop=True), reads=[kT, qm[p]], writes=[LG]))
                r.append(lambda: S.op("act", lambda e: e.activation(out=E_[:].rearrange("p h t -> p (h t)"), in_=LG[:], func=AF.Exp, scale=0.125),
                                      reads=[LG], writes=[E_]))
                return r

            def s2(kb):
                E_ = E[kb % 2]
                P_ = Pm[kb % 2]
                r = []
                r.append(lambda: S.op("dve", lambda e: e.tensor_tensor(out=P_[:], in0=E_[:], in1=PT[:, kb % 2, :].unsqueeze(1).to_broadcast([128, 4, 128]),
                                                                      op=ALU.mult), reads=[E_, PT], writes=[P_]))
                slot = kb - (2 * j - 1)
                if slot >= 0:
                    nr = near[p][slot]
                    r.append(lambda: S.op("dve", lambda e: e.tensor_tensor(out=P_[:], in0=P_[:], in1=nr[:], op=ALU.mult), reads=[P_, nr], writes=[P_]))
                for h in range(4):
                    r.append(lambda h=h: S.op("pe", lambda e: e.matmul(OB[h][:, 0:65], lhsT=P_[:, h, :], rhs=vx[:, kb, h * 65:(h + 1) * 65],
                                                                       start=(kb == 0), stop=(kb == nk - 1)), reads=[P_, vx], writes=[OB[h]]))
                return r

            ops += s1(0)
            for kb in range(nk):
                if kb + 1 < nk:
                    ops += s1(kb + 1)
                ops += s2(kb)
            for h in range(4):
                ops.append(lambda h=h: S.op("dve", lambda e: e.reciprocal(out=rcp[:, h:h + 1], in_=OB[h][:, 64:65]), reads=[OB[h]], writes=[rcp]))
                ops.append(lambda h=h: S.op("dve", lambda e: e.tensor_scalar(out=o_n[:, h * 64:(h + 1) * 64], in0=OB[h][:, 0:64], scalar1=rcp[:, h:h + 1],
                                                                            scalar2=None, op0=ALU.mult), reads=[OB[h], rcp], writes=[o_n]))
            ob = oTb[j % 2]
            for c in range(2):
                ops.append(lambda c=c: S.op("pe", lambda e: e.transpose(PT[:, c, :], o_n[:, c * 128:(c + 1) * 128], C.ident[:]), reads=[o_n, C.ident], writes=[PT]))
                ops.append(lambda c=c: S.op("act", lambda e: e.copy(out=ob[:, c, :], in_=PT[:, c, :]), reads=[PT], writes=[ob]))
            ops.append(lambda: S.dma("sp", och[j % 2], ov[:, :, j * 128:(j + 1) * 128], ob[:], reads=[ob]))
            return ops

        def merge(a, b):
            na, nb = len(a), len(b)
            ia = ib = 0
            while ia < na or ib < nb:
                if ib >= nb or (ia < na and ia * nb <= ib * na):
                    a[ia]()
                    ia += 1
                else:
                    b[ib]()
                    ib += 1

        merge(ops_A(0) + ops_B(0), [])
        for j in range(nj):
            if j + 1 < nj:
                merge(ops_A(j + 1) + ops_B(j + 1), ops_D(j))
            else:
                merge([], ops_D(j))
        S.barrier()


ATTN_IMPL = phase_attn2
```
